# Optimizing a Trainium2 kernel written in Bass

```python
import jax, jax.numpy as jnp
from jax import lax
import numpy as np

D_MODEL = 1024
BATCH = 4
SEQ = 4096
DEPTH = 4

N_META = 16
N_MIXERS = 2
RMS_EPS = 1e-6
ROPE_THETA = 10000.0
ATT_HEADS = 16
ATT_KV_HEADS = 4
ATT_HEAD_DIM = D_MODEL // ATT_HEADS
ATT_GROUP = ATT_HEADS // ATT_KV_HEADS
IDX_HEADS = 8
IDX_DIM = 128
TOPK_MAX = 256
Q_BLOCK = 128
DSA_SPLITS = [ATT_HEADS * ATT_HEAD_DIM, ATT_KV_HEADS * ATT_HEAD_DIM, ATT_KV_HEADS * ATT_HEAD_DIM,
              IDX_HEADS * IDX_DIM, IDX_DIM, IDX_HEADS]
DSA_IN = sum(DSA_SPLITS)
M_HEADS = 4
M_QK_DIM = D_MODEL // (2 * M_HEADS)
M_V_DIM = D_MODEL // M_HEADS
M_CHUNK = 64
GATE_CAP = 15.0
MLSTM_SPLITS = [M_HEADS * M_QK_DIM, M_HEADS * M_QK_DIM, M_HEADS * M_V_DIM, M_HEADS * M_V_DIM,
                M_HEADS, M_HEADS]
MLSTM_IN = sum(MLSTM_SPLITS)
D_FF = 7 * D_MODEL // 2
N_EXPERTS = 8
TOP_K_EXPERTS = 2
N_A = (DEPTH + 1) // 2
N_B = DEPTH // 2

kernel_name = "hybrid_dsa_mlstm_moe_trunk"


def rms_norm(x, g):
    xf = x.astype(jnp.float32)
    y = xf * lax.rsqrt(jnp.mean(xf * xf, axis=-1, keepdims=True) + RMS_EPS)
    return (y * g.astype(jnp.float32)).astype(x.dtype)


def rope(x, pos):
    half = x.shape[-1] // 2
    inv = ROPE_THETA ** (-jnp.arange(half, dtype=jnp.float32) / half)
    ang = pos.astype(jnp.float32)[:, None] * inv[None, :]
    cos = jnp.cos(ang)[:, None, :]
    sin = jnp.sin(ang)[:, None, :]
    x1 = x[..., :half].astype(jnp.float32)
    x2 = x[..., half:].astype(jnp.float32)
    return jnp.concatenate([x1 * cos - x2 * sin, x2 * cos + x1 * sin], axis=-1).astype(x.dtype)


def split_cols(a, sizes):
    return jnp.split(a, list(np.cumsum(sizes)[:-1]), axis=-1)


def dsa_mixer(h, w_in, g_q, g_k, w_out, top_k):
    B, L, _ = h.shape
    q, k, v, iq, ik, iw = split_cols(h @ w_in, DSA_SPLITS)
    pos = jnp.arange(L)
    q = rope(rms_norm(q.reshape(B, L, ATT_HEADS, ATT_HEAD_DIM), g_q), pos)
    k = rope(rms_norm(k.reshape(B, L, ATT_KV_HEADS, ATT_HEAD_DIM), g_k), pos)
    v = v.reshape(B, L, ATT_KV_HEADS, ATT_HEAD_DIM)
    iq = rope(iq.reshape(B, L, IDX_HEADS, IDX_DIM), pos)
    ik = rope(ik[:, :, None, :], pos)[:, :, 0]
    iw = iw * (IDX_HEADS ** -0.5 * IDX_DIM ** -0.5)
    n_blocks = -(-L // Q_BLOCK)
    pad = n_blocks * Q_BLOCK - L
    padq = lambda a: jnp.pad(a, [(0, 0), (0, pad)] + [(0, 0)] * (a.ndim - 2))
    qp = padq(q).reshape(B, n_blocks * Q_BLOCK, ATT_KV_HEADS, ATT_GROUP, ATT_HEAD_DIM)
    iqp, iwp = padq(iq), padq(iw)
    key_pos = jnp.arange(L)
    scale = ATT_HEAD_DIM ** -0.5

    def block(bi):
        start = bi * Q_BLOCK
        qb = lax.dynamic_slice_in_dim(qp, start, Q_BLOCK, axis=1)
        iqb = lax.dynamic_slice_in_dim(iqp, start, Q_BLOCK, axis=1)
        iwb = lax.dynamic_slice_in_dim(iwp, start, Q_BLOCK, axis=1)
        qpos = start + jnp.arange(Q_BLOCK)
        causal = key_pos[None, :] <= qpos[:, None]
        s = jnp.einsum('bqhd,bsd->bqhs', iqb, ik, preferred_element_type=jnp.float32)
        score = jnp.einsum('bqhs,bqh->bqs', jax.nn.relu(s), iwb.astype(jnp.float32))
        score = jnp.where(causal[None], score, -jnp.inf)
        _, sel = lax.top_k(score, top_k)
        valid = sel <= qpos[None, :, None]
        k_sel = jax.vmap(lambda kk, ii: kk[ii])(k, sel)
        v_sel = jax.vmap(lambda vv, ii: vv[ii])(v, sel)
        logits = jnp.einsum('bqgrd,bqkgd->bqgrk', qb, k_sel, preferred_element_type=jnp.float32) * scale
        logits = jnp.where(valid[:, :, None, None, :], logits, -jnp.inf)
        p = jax.nn.softmax(logits, axis=-1)
        return jnp.einsum('bqgrk,bqkgd->bqgrd', p.astype(v.dtype), v_sel)

    out = lax.map(block, jnp.arange(n_blocks))
    out = out.transpose(1, 0, 2, 3, 4, 5).reshape(B, n_blocks * Q_BLOCK, D_MODEL)[:, :L]
    return out @ w_out


def mlstm_mixer(h, w_in, b_i, b_f, g_out, w_out):
    B, L, _ = h.shape
    q, k, v, o, ig, fg = split_cols(h @ w_in, MLSTM_SPLITS)
    f32 = jnp.float32
    q = q.reshape(B, L, M_HEADS, M_QK_DIM).astype(f32)
    k = k.reshape(B, L, M_HEADS, M_QK_DIM).astype(f32) * (M_QK_DIM ** -0.5)
    v = v.reshape(B, L, M_HEADS, M_V_DIM).astype(f32)
    log_i = GATE_CAP * jnp.tanh((ig.astype(f32) + b_i.astype(f32)) / GATE_CAP)
    log_f = jax.nn.log_sigmoid(GATE_CAP * jnp.tanh((fg.astype(f32) + b_f.astype(f32)) / GATE_CAP))
    pf = (-N_META) % M_CHUNK
    pb = (-(pf + L)) % M_CHUNK
    Lp = pf + L + pb
    nc = Lp // M_CHUNK
    padt = lambda a, c=0.0: jnp.pad(a, [(0, 0), (pf, pb)] + [(0, 0)] * (a.ndim - 2), constant_values=c)
    to_chunks = lambda a: a.reshape(B, nc, M_CHUNK, M_HEADS, -1).transpose(1, 0, 3, 2, 4)
    gchunks = lambda a: a.reshape(B, nc, M_CHUNK, M_HEADS).transpose(1, 0, 3, 2)
    qc, kc, vc = to_chunks(padt(q)), to_chunks(padt(k)), to_chunks(padt(v))
    lic = gchunks(padt(log_i, -jnp.inf))
    lfc = gchunks(padt(log_f, 0.0))
    tril = jnp.tril(jnp.ones((M_CHUNK, M_CHUNK), dtype=bool))

    def step(carry, xs):
        C_st, n_st, m_st = carry
        qx, kx, vx, li, lf = xs
        b = jnp.cumsum(lf, axis=-1)
        dmat = jnp.where(tril, b[..., :, None] - b[..., None, :] + li[..., None, :], -jnp.inf)
        inter = b + m_st[..., None]
        m_t = jnp.maximum(inter, jnp.max(dmat, axis=-1))
        w_inter = jnp.exp(inter - m_t)
        s = jnp.einsum('bhtd,bhsd->bhts', qx, kx) * jnp.exp(dmat - m_t[..., None])
        num = w_inter[..., None] * jnp.einsum('bhvd,bhtd->bhtv', C_st, qx) + jnp.einsum('bhts,bhsv->bhtv', s, vx)
        den = w_inter * jnp.einsum('bhd,bhtd->bht', n_st, qx) + jnp.sum(s, axis=-1)
        hout = num / jnp.maximum(jnp.abs(den), jnp.exp(-m_t))[..., None]
        b_last = b[..., -1]
        log_w = b_last[..., None] - b + li
        m_new = jnp.maximum(b_last + m_st, jnp.max(log_w, axis=-1))
        decay = jnp.exp(b_last + m_st - m_new)
        wk = jnp.exp(log_w - m_new[..., None])
        C_new = decay[..., None, None] * C_st + jnp.einsum('bhs,bhsv,bhsd->bhvd', wk, vx, kx)
        n_new = decay[..., None] * n_st + jnp.einsum('bhs,bhsd->bhd', wk, kx)
        return (C_new, n_new, m_new), hout

    init = (jnp.zeros((B, M_HEADS, M_V_DIM, M_QK_DIM), f32), jnp.zeros((B, M_HEADS, M_QK_DIM), f32),
            jnp.zeros((B, M_HEADS), f32))
    _, hs = lax.scan(step, init, (qc, kc, vc, lic, lfc))
    hs = hs.transpose(1, 0, 3, 2, 4).reshape(B, Lp, M_HEADS, M_V_DIM)[:, pf:pf + L]
    hs = rms_norm(hs, g_out.reshape(M_HEADS, M_V_DIM)).reshape(B, L, D_MODEL)
    y = (hs * jax.nn.sigmoid(o.astype(f32))).astype(h.dtype)
    return y @ w_out


def swiglu(h, w_gate, w_up, w_down):
    return (jax.nn.silu(h @ w_gate) * (h @ w_up)) @ w_down


def moe_ffn(h, w_router, w_gate, w_up, w_down):
    logits = (h @ w_router).astype(jnp.float32)
    top_val, top_idx = lax.top_k(logits, TOP_K_EXPERTS)
    gates = jax.nn.softmax(top_val, axis=-1)
    combine = jnp.sum(jax.nn.one_hot(top_idx, N_EXPERTS, dtype=jnp.float32) * gates[..., None], axis=-2)
    y = jnp.zeros_like(h)
    for e in range(N_EXPERTS):
        y = y + combine[..., e:e + 1].astype(h.dtype) * swiglu(h, w_gate[e], w_up[e], w_down[e])
    return y


def setup_inputs(seed: int = 0) -> dict:
    key = jax.random.key(seed)
    ks = jax.random.split(key, 24)
    nrm = lambda k, shape, fan_in: jax.random.normal(k, shape, jnp.float32) * (fan_in ** -0.5)
    gain = lambda k, shape: 1.0 + 0.02 * jax.random.normal(k, shape, jnp.float32)
    out_scale = (2.0 * DEPTH) ** -0.5
    b_f = jnp.linspace(3.0, 6.0, M_HEADS, dtype=jnp.float32)[None, :] + 0.1 * jax.random.normal(ks[9], (N_B, M_HEADS), jnp.float32)
    return {
        "x": jax.random.normal(ks[0], (BATCH, SEQ, D_MODEL), jnp.float32),
        "meta": jax.random.normal(ks[1], (N_META, D_MODEL), jnp.float32),
        "norm_mixer": gain(ks[2], (DEPTH, D_MODEL)),
        "norm_ffn": gain(ks[3], (DEPTH, D_MODEL)),
        "dsa_w_in": nrm(ks[4], (N_A, D_MODEL, DSA_IN), D_MODEL),
        "dsa_q_norm": gain(ks[5], (N_A, ATT_HEAD_DIM)),
        "dsa_k_norm": gain(ks[6], (N_A, ATT_HEAD_DIM)),
        "dsa_w_out": nrm(ks[7], (N_A, D_MODEL, D_MODEL), D_MODEL) * out_scale,
        "mlstm_w_in": nrm(ks[8], (N_B, D_MODEL, MLSTM_IN), D_MODEL),
        "mlstm_b_i": 0.1 * jax.random.normal(ks[10], (N_B, M_HEADS), jnp.float32),
        "mlstm_b_f": b_f,
        "mlstm_out_norm": gain(ks[11], (N_B, D_MODEL)),
        "mlstm_w_out": nrm(ks[12], (N_B, D_MODEL, D_MODEL), D_MODEL) * out_scale,
        "ffn_w_gate": nrm(ks[13], (N_A, D_MODEL, D_FF), D_MODEL),
        "ffn_w_up": nrm(ks[14], (N_A, D_MODEL, D_FF), D_MODEL),
        "ffn_w_down": nrm(ks[15], (N_A, D_FF, D_MODEL), D_FF) * out_scale,
        "moe_router": nrm(ks[16], (N_B, D_MODEL, N_EXPERTS), D_MODEL),
        "moe_w_gate": nrm(ks[17], (N_B, N_EXPERTS, D_MODEL, D_FF), D_MODEL),
        "moe_w_up": nrm(ks[18], (N_B, N_EXPERTS, D_MODEL, D_FF), D_MODEL),
        "moe_w_down": nrm(ks[19], (N_B, N_EXPERTS, D_FF, D_MODEL), D_FF) * out_scale,
    }


def reference(x, meta, norm_mixer, norm_ffn, dsa_w_in, dsa_q_norm, dsa_k_norm, dsa_w_out,
              mlstm_w_in, mlstm_b_i, mlstm_b_f, mlstm_out_norm, mlstm_w_out,
              ffn_w_gate, ffn_w_up, ffn_w_down, moe_router, moe_w_gate, moe_w_up, moe_w_down):
    B, S, D = x.shape
    top_k = min(TOPK_MAX, S // 4)
    h = jnp.concatenate([jnp.broadcast_to(meta[None].astype(x.dtype), (B, N_META, D)), x], axis=1)
    for i in range(DEPTH):
        j = i // N_MIXERS
        hn = rms_norm(h, norm_mixer[i])
        if i % N_MIXERS == 0:
            h = h + dsa_mixer(hn, dsa_w_in[j], dsa_q_norm[j], dsa_k_norm[j], dsa_w_out[j], top_k)
        else:
            h = h + mlstm_mixer(hn, mlstm_w_in[j], mlstm_b_i[j], mlstm_b_f[j], mlstm_out_norm[j], mlstm_w_out[j])
        hn = rms_norm(h, norm_ffn[i])
        if i % 2 == 0:
            h = h + swiglu(hn, ffn_w_gate[j], ffn_w_up[j], ffn_w_down[j])
        else:
            h = h + moe_ffn(hn, moe_router[j], moe_w_gate[j], moe_w_up[j], moe_w_down[j])
    return h[:, N_META:]
```

```python
import numpy as np
from contextlib import ExitStack
import concourse.bass as bass
import concourse.mybir as mybir
from concourse.bass_utils import run_bass_kernel_spmd

F32 = mybir.dt.float32
BF16 = mybir.dt.bfloat16
I32 = mybir.dt.int32
AF = mybir.ActivationFunctionType
ALU = mybir.AluOpType
AX = mybir.AxisListType

D = 1024
NMETA = 16
SEQ = 4096
LTOK = SEQ + NMETA
NT = 34
LP = NT * 128
NTO = 17
DFF = 3584
NE = 8
EPS = 1e-6


class Sched:
    ENG = ("pe", "act", "dve", "pool", "sp")
    NPH = 0

    def __init__(self, nc, es, same_engine_sync=True):
        self.nc = nc
        self.es = es
        self.ops = []
        self.state = {}
        self.same = same_engine_sync
        self.dma_keys = {}

    @staticmethod
    def _norm(res):
        if isinstance(res, tuple):
            return id(res[0]) if not isinstance(res[0], str) else res[0], res[1]
        return (id(res) if not isinstance(res, str) else res), None

    def _conf(self, t, k):
        st = self.state.get(t)
        if not st:
            return []
        if k is None:
            return list(st.values())
        out = []
        if None in st:
            out.append(st[None])
        if k in st:
            out.append(st[k])
        return out

    def add(self, eng, fn, r=(), w=(), dma=None, cc=False):
        idx = len(self.ops)
        deps = set()
        rn = [self._norm(x) for x in r]
        wn = [self._norm(x) for x in w]
        for t, k in rn:
            for e in self._conf(t, k):
                if e[0] is not None:
                    deps.add(e[0])
        for t, k in wn:
            for e in self._conf(t, k):
                if e[0] is not None:
                    deps.add(e[0])
                deps.update(e[1])
        for t, k in rn:
            st = self.state.setdefault(t, {})
            st.setdefault(k, [None, []])[1].append(idx)
        for t, k in wn:
            st = self.state.setdefault(t, {})
            if k is None:
                st.clear()
            st[k] = [idx, []]
        deps.discard(idx)
        if dma is not None:
            dma = self._norm(dma)
        self.ops.append(dict(eng=eng, fn=fn, deps=deps, dma=dma, inc=False, count=None, cc=cc))
        return idx

    def _skip(self, dep, op):
        if dep["dma"] is not None:
            return False
        if dep["eng"] == op["eng"]:
            if dep["eng"] == "pe" and op["dma"] is None:
                return True
            if not self.same:
                return True
        return False

    def emit(self):
        nc, es, ops = self.nc, self.es, self.ops
        for op in ops:
            for d in op["deps"]:
                dep = ops[d]
                if dep["dma"] is None and not self._skip(dep, op):
                    dep["inc"] = True
        cnt = {e: 0 for e in self.ENG}
        dcnt = {}
        for op in ops:
            if op["dma"] is not None:
                dcnt[op["dma"]] = dcnt.get(op["dma"], 0) + (1 if op["cc"] else 16)
                op["count"] = dcnt[op["dma"]]
            elif op["inc"]:
                cnt[op["eng"]] += 1
                op["count"] = cnt[op["eng"]]
        Sched.NPH += 1
        pfx = "p%d_" % Sched.NPH
        sems = {e: nc.alloc_semaphore(name=pfx + "s_" + e) for e in self.ENG}
        dsem = {}
        for i, k in enumerate(dcnt):
            dsem[k] = nc.alloc_semaphore(name=pfx + "d%d" % i)
        self.n_sems = len(sems) + len(dsem)

        def run(engname, e):
            waited = {}
            for op in ops:
                if op["eng"] != engname:
                    continue
                need = {}
                for d in op["deps"]:
                    dep = ops[d]
                    if self._skip(dep, op):
                        continue
                    if dep["dma"] is not None:
                        key, sem = ("d", dep["dma"]), dsem[dep["dma"]]
                    else:
                        key, sem = ("e", dep["eng"]), sems[dep["eng"]]
                    if need.get(key, (None, 0))[1] < dep["count"]:
                        need[key] = (sem, dep["count"])
                for key, (sem, val) in need.items():
                    if waited.get(key, 0) >= val:
                        continue
                    e.wait_ge(sem, val)
                    waited[key] = val
                ins = op["fn"](e)
                if op["dma"] is not None:
                    if op["cc"]:
                        ins.then_inc(dsem[op["dma"]])
                    else:
                        ins.then_inc(dsem[op["dma"]], 16)
                elif op["inc"]:
                    ins.then_inc(sems[op["eng"]], 1)
            if engname == "sp":
                for k, v in dcnt.items():
                    e.wait_ge(dsem[k], v)

        with nc.Block() as block:
            @block.tensor
            def _(e):
                run("pe", e)

            @block.scalar
            def _(e):
                run("act", e)

            @block.vector
            def _(e):
                run("dve", e)

            @block.gpsimd
            def _(e):
                run("pool", e)

            @block.sync
            def _(e):
                run("sp", e)


class Ctx:
    NCTX = 0

    def __init__(self, nc, es):
        self.nc = nc
        self.es = es
        self.S = Sched(nc, es)
        self.n = 0
        Ctx.NCTX += 1
        self.pfx = "c%d_" % Ctx.NCTX
        self.bank = [es.enter_context(nc.psum_tensor(self.pfx + "bank%d" % i, [128, 512], F32)) for i in range(8)]

    def sb(self, shape, dt, name=None):
        self.n += 1
        return self.es.enter_context(self.nc.sbuf_tensor(self.pfx + (name or ("t%d" % self.n)), list(shape), dt))

    def consts(self):
        S = self.S
        self.ones32 = self.sb([128, 128], F32, "ones32")
        self.ident32 = self.sb([128, 128], F32, "ident32")
        self.identb = self.sb([128, 128], BF16, "identb")
        S.add("pool", lambda e: e.memset(self.ones32[:], 1.0), w=[self.ones32])
        S.add("pool", lambda e: e.affine_select(self.ident32[:], self.ones32[:], [[-1, 128]],
                                                ALU.is_equal, 0.0, base=0, channel_multiplier=1),
              r=[self.ones32], w=[self.ident32])
        S.add("pool", lambda e: e.tensor_copy(self.identb[:], self.ident32[:]),
              r=[self.ident32], w=[self.identb])


def rmsnorm_tile(C, h_ap, h_res, g_bc, outs, scr):
    S = C.S
    junk, ss, sq, rstd = scr["junk"], scr["ss"], scr["sq"], scr["rstd"]
    S.add("act", lambda e: e.activation(junk[:], h_ap, AF.Square, accum_out=ss[:]),
          r=[h_res], w=[junk, ss])
    S.add("act", lambda e: e.activation(sq[:], ss[:], AF.Sqrt, bias=scr["eps"][:], scale=1.0 / D),
          r=[ss, scr["eps"]], w=[sq])
    S.add("dve", lambda e: e.reciprocal(rstd[:], sq[:]), r=[sq], w=[rstd])
    for ap, res in outs:
        S.add("dve", lambda e, ap=ap: e.scalar_tensor_tensor(ap, h_ap, rstd[:, 0:1], g_bc[:],
                                                             ALU.mult, ALU.mult),
              r=[h_res, rstd, g_bc], w=[res])


def norm_scratch(C):
    scr = dict(junk=C.sb([128, D], F32), ss=C.sb([128, 1], F32), sq=C.sb([128, 1], F32),
               rstd=C.sb([128, 1], F32), eps=C.sb([128, 1], F32))
    C.S.add("pool", lambda e: e.memset(scr["eps"][:], EPS), w=[scr["eps"]])
    return scr


def ffn_phase(C, h_src, out_dst, g_dram, experts, router, ntile=NTO, h_key="h", out_key="out",
              rowmask=None, after=None):
    nc, S = C.nc, C.S
    NTOK = ntile * 128
    moe = router is not None
    GC = 2
    NG = DFF // (128 * GC)
    yacc = C.sb([128, ntile, D], F32, "yacc")
    hnT = C.sb([128, 8, NTOK], BF16, "hnT")
    g_bc = C.sb([128, D], F32, "g_bc")
    scr = norm_scratch(C)
    hnb = [C.sb([128, D], BF16, "hnb%d" % i) for i in range(2)]
    S.add("sp", lambda e: e.dma_start(out=g_bc[:], in_=g_dram.partition_broadcast(128)),
          w=[g_bc], dma=g_bc)
    if moe:
        hn32 = [C.sb([128, D], F32, "hn32_%d" % i) for i in range(2)]
        hnT32 = [C.sb([128, 8, 128], F32, "hnT32_%d" % i) for i in range(2)]
        wr = C.sb([128, 8, NE], F32, "wr")
        call = C.sb([128, ntile, NE], F32, "call")
        rt = dict(lg=C.sb([128, NE], F32), m8=C.sb([128, 8], F32), negm=C.sb([128, 1], F32),
                  mask=C.sb([128, NE], F32), ex=C.sb([128, NE], F32), p=C.sb([128, NE], F32),
                  den=C.sb([128, 1], F32), rden=C.sb([128, 1], F32))
        S.add("sp", lambda e: e.dma_start(out=wr[:], in_=router.rearrange("(kc p) n -> p kc n", p=128)),
              w=[wr], dma=wr)

    for ti in range(ntile):
        hb = hnb[ti % 2]
        S.add("sp", lambda e, ti=ti: e.dma_start(out=yacc[:, ti, :], in_=h_src[ti * 128:(ti + 1) * 128, :]),
              r=[(h_key, ti)], w=[(yacc, ti)], dma=(yacc, ti))
        outs = [(hb[:], hb)]
        if moe:
            outs.append((hn32[ti % 2][:], hn32[ti % 2]))
        rmsnorm_tile(C, yacc[:, ti, :], (yacc, ti), g_bc, outs, scr)
        pb = C.bank[4 + 2 * (ti % 2)]
        pbv = pb[:].bitcast(BF16)
        for kc in range(8):
            S.add("pe", lambda e, kc=kc, hb=hb, pbv=pbv: e.transpose(
                pbv[:, kc * 128:(kc + 1) * 128], hb[:, kc * 128:(kc + 1) * 128], C.identb[:]),
                r=[hb, C.identb], w=[pb])
        S.add("act", lambda e, ti=ti, pbv=pbv: e.activation(
            hnT[:, :, ti * 128:(ti + 1) * 128], pbv.rearrange("p (a b) -> p a b", a=8), AF.Copy),
            r=[pb], w=[(hnT, ti)])
        if moe:
            h32, hT32 = hn32[ti % 2], hnT32[ti % 2]
            pa, pbk = C.bank[0 + 2 * (ti % 2)], C.bank[1 + 2 * (ti % 2)]
            for kc in range(8):
                bk = pa if kc < 4 else pbk
                S.add("pe", lambda e, kc=kc, bk=bk, h32=h32: e.transpose(
                    bk[:, (kc % 4) * 128:(kc % 4 + 1) * 128], h32[:, kc * 128:(kc + 1) * 128], C.ident32[:]),
                    r=[h32, C.ident32], w=[bk])
            S.add("dve", lambda e, pa=pa, hT32=hT32: e.tensor_copy(
                hT32[:, 0:4, :], pa[:].rearrange("p (a b) -> p a b", a=4)), r=[pa], w=[(hT32, 0)])
            S.add("dve", lambda e, pbk=pbk, hT32=hT32: e.tensor_copy(
                hT32[:, 4:8, :], pbk[:].rearrange("p (a b) -> p a b", a=4)), r=[pbk], w=[(hT32, 1)])
            lgp = C.bank[4 + 2 * (ti % 2) + 1]
            for kc in range(8):
                S.add("pe", lambda e, kc=kc, hT32=hT32, lgp=lgp: e.matmul(
                    lgp[:, 0:NE], hT32[:, kc, :], wr[:, kc, :], start=(kc == 0), stop=(kc == 7)),
                    r=[hT32, wr], w=[lgp])
            lg, m8, negm, mask, ex, p, den, rden = (rt[k] for k in
                                                    ("lg", "m8", "negm", "mask", "ex", "p", "den", "rden"))
            S.add("dve", lambda e, lgp=lgp: e.tensor_copy(lg[:], lgp[:, 0:NE]), r=[lgp], w=[lg])
            S.add("dve", lambda e: e.max(m8[:], lg[:]), r=[lg], w=[m8])
            S.add("dve", lambda e: e.tensor_scalar(negm[:], m8[:, 0:1], -1.0, None, ALU.mult),
                  r=[m8], w=[negm])
            S.add("dve", lambda e: e.tensor_scalar(mask[:], lg[:], m8[:, 1:2], None, ALU.is_ge),
                  r=[lg, m8], w=[mask])
            S.add("act", lambda e: e.activation(ex[:], lg[:], AF.Exp, bias=negm[:], scale=1.0),
                  r=[lg, negm], w=[ex])
            S.add("dve", lambda e: e.tensor_tensor(p[:], ex[:], mask[:], ALU.mult), r=[ex, mask], w=[p])
            S.add("dve", lambda e: e.tensor_reduce(den[:], p[:], AX.X, ALU.add), r=[p], w=[den])
            S.add("dve", lambda e: e.reciprocal(rden[:], den[:]), r=[den], w=[rden])
            S.add("dve", lambda e, ti=ti: e.tensor_scalar(call[:, ti, :], p[:], rden[:, 0:1], None, ALU.mult),
                  r=[p, rden], w=[(call, ti)])

    NWB = 3
    wgt = [C.sb([128, 8, GC * 128], BF16, "wg%d" % i) for i in range(NWB)]
    wut = [C.sb([128, 8, GC * 128], BF16, "wu%d" % i) for i in range(NWB)]
    wdt = [C.sb([128, GC, D], BF16, "wd%d" % i) for i in range(NWB)]
    actT = [C.sb([128, GC, NTOK], BF16, "actT%d" % i) for i in range(2)]
    sgt = [C.sb([128, 512], F32, "sg%d" % i) for i in range(2)]
    PG = [C.bank[0], C.bank[1]]
    PU = [C.bank[2], C.bank[3]]
    PD = [(C.bank[4], C.bank[5]), (C.bank[6], C.bank[7])]
    tblocks = []
    t0 = 0
    while t0 < NTOK:
        tblocks.append((t0, min(512, NTOK - t0)))
        t0 += 512
    gi_glob = 0
    blk = 0
    dcount = 0
    for ei, (wg, wu, wd) in enumerate(experts):
        for gi in range(NG):
            c0 = gi * GC * 128
            wb = gi_glob % NWB
            ab = gi_glob % 2
            gi_glob += 1
            wgb, wub, wdb, at = wgt[wb], wut[wb], wdt[wb], actT[ab]
            S.add("pool", lambda e, wgb=wgb, wg=wg, c0=c0: e.dma_start(
                out=wgb[:], in_=wg[:, c0:c0 + GC * 128].rearrange("(kc p) c -> p kc c", p=128)),
                w=[wgb], dma=wgb)
            S.add("pool", lambda e, wub=wub, wu=wu, c0=c0: e.dma_start(
                out=wub[:], in_=wu[:, c0:c0 + GC * 128].rearrange("(kc p) c -> p kc c", p=128)),
                w=[wub], dma=wub)
            S.add("pool", lambda e, wdb=wdb, wd=wd, c0=c0: e.dma_start(
                out=wdb[:], in_=wd[c0:c0 + GC * 128, :].rearrange("(j p) n -> p j n", p=128)),
                w=[wdb], dma=wdb)
            for ci in range(GC):
                for (tb0, tbn) in tblocks:
                    b = blk % 2
                    blk += 1
                    pg, pu, sg = PG[b], PU[b], sgt[b]
                    for kc in range(8):
                        S.add("pe", lambda e, pg=pg, wgb=wgb, ci=ci, kc=kc, tb0=tb0, tbn=tbn: e.matmul(
                            pg[:, 0:tbn], wgb[:, kc, ci * 128:(ci + 1) * 128], hnT[:, kc, tb0:tb0 + tbn],
                            start=(kc == 0), stop=(kc == 7)), r=[wgb, hnT], w=[pg])
                    for kc in range(8):
                        S.add("pe", lambda e, pu=pu, wub=wub, ci=ci, kc=kc, tb0=tb0, tbn=tbn: e.matmul(
                            pu[:, 0:tbn], wub[:, kc, ci * 128:(ci + 1) * 128], hnT[:, kc, tb0:tb0 + tbn],
                            start=(kc == 0), stop=(kc == 7)), r=[wub, hnT], w=[pu])
                    S.add("act", lambda e, pg=pg, sg=sg, tbn=tbn: e.activation(
                        sg[:, 0:tbn], pg[:, 0:tbn], AF.Silu), r=[pg], w=[sg])
                    S.add("dve", lambda e, at=at, ci=ci, tb0=tb0, tbn=tbn, sg=sg, pu=pu: e.tensor_tensor(
                        at[:, ci, tb0:tb0 + tbn], sg[:, 0:tbn], pu[:, 0:tbn], ALU.mult),
                        r=[sg, pu], w=[(at, (ci, tb0))])
            for ti in range(ntile):
                pd = PD[dcount % 2]
                dcount += 1
                for half in range(2):
                    for ci in range(GC):
                        S.add("pe", lambda e, pd=pd, half=half, ci=ci, at=at, wdb=wdb, ti=ti: e.matmul(
                            pd[half][:, :], at[:, ci, ti * 128:(ti + 1) * 128],
                            wdb[:, ci, half * 512:(half + 1) * 512],
                            start=(ci == 0), stop=(ci == GC - 1)), r=[at, wdb], w=[pd[half]])
                for half in range(2):
                    ysl = yacc[:, ti, half * 512:(half + 1) * 512]
                    if moe:
                        S.add("dve", lambda e, ysl=ysl, pd=pd, half=half, ti=ti, ei=ei: e.scalar_tensor_tensor(
                            ysl, pd[half][:, :], call[:, ti, ei:ei + 1], ysl, ALU.mult, ALU.add),
                            r=[pd[half], (call, ti), (yacc, ti)], w=[(yacc, ti)])
                    else:
                        S.add("dve", lambda e, ysl=ysl, pd=pd, half=half: e.tensor_tensor(
                            ysl, pd[half][:, :], ysl, ALU.add),
                            r=[pd[half], (yacc, ti)], w=[(yacc, ti)])
    if rowmask is not None:
        rm = C.sb([128, 1], F32, "rowmask")
        S.add("sp", lambda e: e.dma_start(out=rm[:], in_=rowmask), w=[rm], dma=rm)
        S.add("dve", lambda e: e.tensor_scalar(yacc[:, ntile - 1, :], yacc[:, ntile - 1, :], rm[:, 0:1], None,
                                               ALU.mult), r=[(yacc, ntile - 1), rm], w=[(yacc, ntile - 1)])
    for ti in range(ntile):
        S.add("sp", lambda e, ti=ti: e.dma_start(out=out_dst[ti * 128:(ti + 1) * 128, :], in_=yacc[:, ti, :]),
              r=[(yacc, ti)], w=[(out_key, ti)], dma=(yacc, ti))
    if after is not None:
        after(out_key)


def mm(C, out_ap, lhsT, rhs, start, stop, r, w):
    C.S.add("pe", lambda e: e.matmul(out_ap, lhsT, rhs, start=start, stop=stop), r=r, w=w)


def load_norm_T(C, src_ap, src_key, g_bc, scr, hbuf, hnb, hnT, tbank, dkey):
    S = C.S
    S.add("sp", lambda e: e.dma_start(out=hbuf[:], in_=src_ap), r=[src_key], w=[hbuf], dma=dkey)
    rmsnorm_tile(C, hbuf[:], hbuf, g_bc, [(hnb[:], hnb)], scr)
    transpose_to(C, hnb, hnT, 8, tbank)


def transpose_to(C, src, dstT, n, tbank, eng="act"):
    S = C.S
    pbv = tbank[:].bitcast(BF16)
    for kc in range(n):
        S.add("pe", lambda e, kc=kc: e.transpose(pbv[:, kc * 128:(kc + 1) * 128],
                                                  src[:, kc * 128:(kc + 1) * 128], C.identb[:]),
              r=[src, C.identb], w=[tbank])
    if eng == "act":
        S.add("act", lambda e: e.activation(dstT[:, 0:n, :], pbv[:, 0:n * 128].rearrange("p (a b) -> p a b", a=n),
                                            AF.Copy), r=[tbank], w=[dstT])
    else:
        S.add("dve", lambda e: e.tensor_copy(dstT[:, 0:n, :], pbv[:, 0:n * 128].rearrange("p (a b) -> p a b", a=n)),
              r=[tbank], w=[dstT])


def load_weight_bf16(C, dst, src, ncols, key):
    c0 = 0
    while c0 < ncols:
        n = min(1536, ncols - c0)
        C.S.add("pool", lambda e, c0=c0, n=n: e.dma_start(
            out=dst[:, :, c0:c0 + n], in_=src[:, c0:c0 + n].rearrange("(kc p) c -> p kc c", p=128)),
            w=[(dst, c0)], dma=(dst, c0))
        c0 += n


def mlstm_phase(C, h_full, h_own, out_dst, g_dram, w_in, b_i, b_f, g_out, w_out, M0d, M1d, nstep=NTO):
    S = C.S
    bank = C.bank
    W = C.sb([128, 8, 3080], BF16, "mW")
    Wo = C.sb([128, 8, D], BF16, "mWo")
    load_weight_bf16(C, W, w_in, 3080, "mW")
    load_weight_bf16(C, Wo, w_out, D, "mWo")
    g_bc = C.sb([128, D], F32, "m_gbc")
    go_bc = C.sb([128, D], F32, "m_gobc")
    bb = C.sb([128, 8], F32, "m_bb")
    M = [C.sb([128, 128], F32, "m_M%d" % i) for i in range(2)]
    S.add("sp", lambda e: e.dma_start(out=g_bc[:], in_=g_dram.partition_broadcast(128)), w=[g_bc], dma=g_bc)
    S.add("sp", lambda e: e.dma_start(out=go_bc[:], in_=g_out.partition_broadcast(128)), w=[go_bc], dma=go_bc)
    S.add("sp", lambda e: e.dma_start(out=bb[:, 0:4], in_=b_i.partition_broadcast(128)), w=[(bb, 0)], dma=(bb, 0))
    S.add("sp", lambda e: e.dma_start(out=bb[:, 4:8], in_=b_f.partition_broadcast(128)), w=[(bb, 1)], dma=(bb, 1))
    S.add("sp", lambda e: e.dma_start(out=M[0][:], in_=M0d), w=[M[0]], dma=M[0])
    S.add("sp", lambda e: e.dma_start(out=M[1][:], in_=M1d), w=[M[1]], dma=M[1])
    tri = C.sb([128, 128], F32, "m_tri")
    S.add("pool", lambda e: e.affine_select(tri[:], C.ones32[:], [[1, 128]], ALU.is_ge, 0.0, base=0,
                                            channel_multiplier=-1), r=[C.ones32], w=[tri])
    c0t = C.sb([128, 1], F32, "m_c0")
    onet = C.sb([128, 1], F32, "m_one")
    S.add("pool", lambda e: e.memset(c0t[:], float(-0.5 * np.log(128.0))), w=[c0t])
    S.add("pool", lambda e: e.memset(onet[:], 1.0), w=[onet])
    scr = norm_scratch(C)
    CT = [C.sb([128, 257], F32, "m_CT%d" % h) for h in range(4)]
    CTb = [C.sb([128, 257], BF16, "m_CTb%d" % h) for h in range(4)]
    for h in range(4):
        S.add("pool", lambda e, h=h: e.memset(CT[h][:], 0.0), w=[CT[h]])
        S.add("pool", lambda e, h=h: e.memset(CTb[h][:], 0.0), w=[CTb[h]])
    vext = [C.sb([128, 4, 257], BF16, "m_vext%d" % x) for x in range(2)]
    for x in range(2):
        S.add("pool", lambda e, x=x: e.memset(vext[x][:, :, 256:257], 1.0), w=[(vext[x], "one")])
    hk = [C.sb([128, D], F32, "m_hk%d" % x) for x in range(2)]
    hnbk = [C.sb([128, D], BF16, "m_hnbk%d" % x) for x in range(2)]
    hnTk = [C.sb([128, 8, 128], BF16, "m_hnTk%d" % x) for x in range(2)]
    kb = [C.sb([128, 512], BF16, "m_kb%d" % x) for x in range(2)]
    kT = [C.sb([128, 4, 128], BF16, "m_kT%d" % x) for x in range(2)]
    kpp = [C.sb([128, 512], BF16, "m_kpp%d" % x) for x in range(2)]
    gx = [C.sb([128, 8], F32, "m_gx%d" % x) for x in range(2)]
    tg = [C.sb([128, 8], F32, "m_tg%d" % x) for x in range(2)]
    ef = [C.sb([128, 4], F32, "m_ef%d" % x) for x in range(2)]
    spl = [C.sb([128, 4], F32, "m_sp%d" % x) for x in range(2)]
    lf = [C.sb([128, 4], F32, "m_lf%d" % x) for x in range(2)]
    dd = [C.sb([128, 4], F32, "m_dd%d" % x) for x in range(2)]
    dd2 = [C.sb([128, 4], F32, "m_dd2%d" % x) for x in range(2)]
    wexp = [C.sb([128, 4], F32, "m_wexp%d" % x) for x in range(2)]
    wst = [C.sb([128, 4], F32, "m_wst%d" % x) for x in range(2)]
    Sm = [C.sb([128, 128], BF16, "m_Sm%d" % x) for x in range(2)]
    cs = C.sb([128, 32], F32, "m_cs")
    ebl = C.sb([128, 4], F32, "m_ebl")
    ebq = C.sb([128, 4], F32, "m_ebq")
    hq = C.sb([128, D], F32, "m_hq")
    hnbq = C.sb([128, D], BF16, "m_hnbq")
    hnTq = C.sb([128, 8, 128], BF16, "m_hnTq")
    qb = C.sb([128, 512], BF16, "m_qb")
    qT = C.sb([128, 4, 128], BF16, "m_qT")
    so = C.sb([128, D], F32, "m_so")
    sog = C.sb([128, D], F32, "m_sog")
    hout = C.sb([128, D], F32, "m_hout")
    sqv = C.sb([128, D], F32, "m_sqv")
    ss4 = C.sb([128, 4], F32, "m_ss4")
    sq4 = C.sb([128, 4], F32, "m_sq4")
    r4 = C.sb([128, 4], F32, "m_r4")
    yb = C.sb([128, D], BF16, "m_yb")
    yT = C.sb([128, 8, 128], BF16, "m_yT")
    dn = C.sb([128, 1], F32, "m_dn")
    dn2 = C.sb([128, 1], F32, "m_dn2")
    rc = C.sb([128, 1], F32, "m_rc")
    scl = C.sb([128, 1], F32, "m_scl")

    for j in range(nstep):
        for x in range(2):
            ti = 2 * j + x
            load_norm_T(C, h_full(ti), ("hfull", ti), g_bc, scr, hk[x], hnbk[x],
                        hnTk[x], bank[0], hk[x])
            for kc in range(8):
                mm(C, bank[1][:, 0:512], hnTk[x][:, kc, :], W[:, kc, 512:1024], kc == 0, kc == 7,
                   [hnTk[x], W], [bank[1]])
            S.add("act", lambda e, x=x: e.activation(kb[x][:], bank[1][:, 0:512], AF.Copy), r=[bank[1]], w=[kb[x]])
            for half in range(2):
                for kc in range(8):
                    mm(C, bank[2 + half][:, 0:512], hnTk[x][:, kc, :],
                       W[:, kc, 1024 + half * 512:1024 + (half + 1) * 512], kc == 0, kc == 7,
                       [hnTk[x], W], [bank[2 + half]])
                S.add("act", lambda e, x=x, half=half: e.activation(
                    vext[x][:, 2 * half:2 * half + 2, 0:256],
                    bank[2 + half][:, 0:512].rearrange("p (a b) -> p a b", a=2), AF.Copy),
                    r=[bank[2 + half]], w=[(vext[x], half)])
            for kc in range(8):
                mm(C, bank[4][:, 0:8], hnTk[x][:, kc, :], W[:, kc, 3072:3080], kc == 0, kc == 7,
                   [hnTk[x], W], [bank[4]])
            S.add("dve", lambda e, x=x: e.tensor_tensor(gx[x][:], bank[4][:, 0:8], bb[:], ALU.add),
                  r=[bank[4], bb], w=[gx[x]])
            S.add("act", lambda e, x=x: e.activation(tg[x][:], gx[x][:], AF.Tanh, scale=1.0 / 15.0),
                  r=[gx[x]], w=[tg[x]])
            S.add("act", lambda e, x=x: e.activation(ef[x][:], tg[x][:, 4:8], AF.Exp, scale=-15.0),
                  r=[tg[x]], w=[ef[x]])
            S.add("act", lambda e, x=x: e.activation(spl[x][:], ef[x][:], AF.Ln, bias=onet[:], scale=1.0),
                  r=[ef[x], onet], w=[spl[x]])
            S.add("dve", lambda e, x=x: e.tensor_scalar(lf[x][:], spl[x][:], -1.0, None, ALU.mult),
                  r=[spl[x]], w=[lf[x]])
            transpose_to(C, kb[x], kT[x], 4, bank[0], eng="dve")
        b5 = bank[5]
        mm(C, b5[:, 0:4], tri[:], lf[0][:], True, True, [tri, lf[0]], [b5])
        mm(C, b5[:, 8:12], C.ones32[:], lf[0][:], True, False, [C.ones32, lf[0]], [b5])
        mm(C, b5[:, 8:12], tri[:], lf[1][:], False, True, [tri, lf[1]], [b5])
        mm(C, b5[:, 16:20], C.ones32[:], lf[0][:], True, False, [C.ones32, lf[0]], [b5])
        mm(C, b5[:, 16:20], C.ones32[:], lf[1][:], False, True, [C.ones32, lf[1]], [b5])
        mm(C, b5[:, 24:28], M[0][:], lf[0][:], True, False, [M[0], lf[0]], [b5])
        mm(C, b5[:, 24:28], M[1][:], lf[1][:], False, True, [M[1], lf[1]], [b5])
        S.add("dve", lambda e: e.tensor_copy(cs[:], b5[:, 0:32]), r=[b5], w=[cs])
        for x in range(2):
            S.add("dve", lambda e, x=x: e.scalar_tensor_tensor(dd[x][:], tg[x][:, 0:4], 15.0, cs[:, 8 * x:8 * x + 4],
                                                               ALU.mult, ALU.subtract),
                  r=[tg[x], cs], w=[dd[x]])
            S.add("act", lambda e, x=x: e.activation(wexp[x][:], dd[x][:], AF.Exp, bias=c0t[:], scale=1.0),
                  r=[dd[x], c0t], w=[wexp[x]])
            S.add("dve", lambda e, x=x: e.tensor_tensor(dd2[x][:], dd[x][:], cs[:, 16:20], ALU.add),
                  r=[dd[x], cs], w=[dd2[x]])
            S.add("act", lambda e, x=x: e.activation(wst[x][:], dd2[x][:], AF.Exp, bias=c0t[:], scale=1.0),
                  r=[dd2[x], c0t], w=[wst[x]])
            for h in range(4):
                S.add("dve", lambda e, x=x, h=h: e.tensor_scalar(
                    kpp[x][:, h * 128:(h + 1) * 128], kb[x][:, h * 128:(h + 1) * 128], wst[x][:, h:h + 1], None,
                    ALU.mult), r=[kb[x], wst[x]], w=[(kpp[x], h)])
        S.add("act", lambda e: e.activation(ebl[:], cs[:, 16:20], AF.Exp), r=[cs], w=[ebl])
        S.add("act", lambda e: e.activation(ebq[:], cs[:, 24:28], AF.Exp), r=[cs], w=[ebq])
        load_norm_T(C, h_own[j * 128:(j + 1) * 128, :], ("hown", j), g_bc, scr, hq, hnbq, hnTq, bank[0], hq)
        for kc in range(8):
            mm(C, bank[1][:, 0:512], hnTq[:, kc, :], W[:, kc, 0:512], kc == 0, kc == 7, [hnTq, W], [bank[1]])
        S.add("act", lambda e: e.activation(qb[:], bank[1][:, 0:512], AF.Copy), r=[bank[1]], w=[qb])
        transpose_to(C, qb, qT, 4, bank[0], eng="dve")
        for half in range(2):
            for kc in range(8):
                mm(C, bank[2 + half][:, 0:512], hnTq[:, kc, :], W[:, kc, 2048 + half * 512:2048 + (half + 1) * 512],
                   kc == 0, kc == 7, [hnTq, W], [bank[2 + half]])
            S.add("act", lambda e, half=half: e.activation(so[:, half * 512:(half + 1) * 512],
                                                           bank[2 + half][:, 0:512], AF.Sigmoid),
                  r=[bank[2 + half]], w=[(so, half)])
        S.add("dve", lambda e: e.tensor_tensor(sog[:], so[:], go_bc[:], ALU.mult), r=[so, go_bc], w=[sog])
        for h in range(4):
            for x in range(2):
                mm(C, bank[6][:, x * 128:(x + 1) * 128], kT[x][:, h, :], qT[:, h, :], True, True,
                   [kT[x], qT], [(bank[6], x)])
                S.add("dve", lambda e, x=x, h=h: e.scalar_tensor_tensor(
                    Sm[x][:], bank[6][:, x * 128:(x + 1) * 128], wexp[x][:, h:h + 1], M[x][:], ALU.mult, ALU.mult),
                    r=[(bank[6], x), wexp[x], M[x]], w=[Sm[x]])
            nb = bank[2 + (h % 2)]
            mm(C, nb[:, 0:257], Sm[0][:], vext[0][:, h, :], True, False, [Sm[0], vext[0]], [nb])
            mm(C, nb[:, 0:257], Sm[1][:], vext[1][:, h, :], False, False, [Sm[1], vext[1]], [nb])
            mm(C, nb[:, 0:257], qT[:, h, :], CTb[h][:], False, True, [qT, CTb[h]], [nb])
            S.add("dve", lambda e, nb=nb, h=h: e.tensor_tensor(dn[:], nb[:, 256:257], ebq[:, h:h + 1], ALU.mult),
                  r=[nb, ebq], w=[dn])
            S.add("dve", lambda e: e.tensor_scalar(dn2[:], dn[:], -1.0, 1.0, ALU.mult, ALU.max), r=[dn], w=[dn2])
            S.add("dve", lambda e: e.tensor_tensor(dn2[:], dn2[:], dn[:], ALU.max), r=[dn, dn2], w=[dn2])
            S.add("dve", lambda e: e.reciprocal(rc[:], dn2[:]), r=[dn2], w=[rc])
            S.add("dve", lambda e, h=h: e.tensor_tensor(scl[:], rc[:], ebq[:, h:h + 1], ALU.mult),
                  r=[rc, ebq], w=[scl])
            S.add("dve", lambda e, nb=nb, h=h: e.tensor_scalar(hout[:, h * 256:(h + 1) * 256], nb[:, 0:256],
                                                               scl[:, 0:1], None, ALU.mult),
                  r=[nb, scl], w=[(hout, h)])
        S.add("act", lambda e: e.activation(sqv[:], hout[:], AF.Square), r=[hout], w=[sqv])
        S.add("dve", lambda e: e.tensor_reduce(ss4[:], sqv[:].rearrange("p (a b) -> p a b", a=4), AX.X, ALU.add),
              r=[sqv], w=[ss4])
        S.add("act", lambda e: e.activation(sq4[:], ss4[:], AF.Sqrt, bias=scr["eps"][:], scale=1.0 / 256.0),
              r=[ss4, scr["eps"]], w=[sq4])
        S.add("dve", lambda e: e.reciprocal(r4[:], sq4[:]), r=[sq4], w=[r4])
        for h in range(4):
            S.add("dve", lambda e, h=h: e.scalar_tensor_tensor(
                yb[:, h * 256:(h + 1) * 256], hout[:, h * 256:(h + 1) * 256], r4[:, h:h + 1],
                sog[:, h * 256:(h + 1) * 256], ALU.mult, ALU.mult), r=[hout, r4, sog], w=[(yb, h)])
        transpose_to(C, yb, yT, 8, bank[0])
        for half in range(2):
            for kc in range(8):
                mm(C, bank[2 + half][:, 0:512], yT[:, kc, :], Wo[:, kc, half * 512:(half + 1) * 512],
                   kc == 0, kc == 7, [yT, Wo], [bank[2 + half]])
            S.add("dve", lambda e, half=half: e.tensor_tensor(hq[:, half * 512:(half + 1) * 512],
                                                              bank[2 + half][:, 0:512],
                                                              hq[:, half * 512:(half + 1) * 512], ALU.add),
                  r=[bank[2 + half], hq], w=[hq])
        S.add("sp", lambda e, j=j: e.dma_start(out=out_dst[j * 128:(j + 1) * 128, :], in_=hq[:]),
              r=[hq], w=[("mout", j)], dma=hq)
        for h in range(4):
            mm(C, bank[7][:, 0:257], kpp[0][:, h * 128:(h + 1) * 128], vext[0][:, h, :], True, False,
               [kpp[0], vext[0]], [bank[7]])
            mm(C, bank[7][:, 0:257], kpp[1][:, h * 128:(h + 1) * 128], vext[1][:, h, :], False, True,
               [kpp[1], vext[1]], [bank[7]])
            S.add("dve", lambda e, h=h: e.scalar_tensor_tensor(CT[h][:], CT[h][:], ebl[:, h:h + 1],
                                                               bank[7][:, 0:257], ALU.mult, ALU.add),
                  r=[CT[h], ebl, bank[7]], w=[CT[h]])
            S.add("act", lambda e, h=h: e.activation(CTb[h][:], CT[h][:], AF.Copy), r=[CT[h]], w=[CTb[h]])


def rope_ops(C, x1, x2, cos, sin, o1, o2, ta, tb, rres, wres, wkey=0):
    S = C.S
    S.add("dve", lambda e: e.tensor_tensor(ta, x1, cos, ALU.mult), r=rres, w=[("ropeta", 0)])
    S.add("dve", lambda e: e.tensor_tensor(tb, x2, sin, ALU.mult), r=rres, w=[("ropetb", 0)])
    S.add("dve", lambda e: e.tensor_tensor(o1, ta, tb, ALU.subtract), r=[("ropeta", 0), ("ropetb", 0)],
          w=[(wres, (wkey, "a"))])
    S.add("dve", lambda e: e.tensor_tensor(ta, x2, cos, ALU.mult), r=rres, w=[("ropeta", 0)])
    S.add("dve", lambda e: e.tensor_tensor(tb, x1, sin, ALU.mult), r=rres, w=[("ropetb", 0)])
    S.add("dve", lambda e: e.tensor_tensor(o2, ta, tb, ALU.add), r=[("ropeta", 0), ("ropetb", 0)],
          w=[(wres, (wkey, "b"))])


def dsa_phase(C, h_full, h_own, out_dst, g_dram, w_in, gq_d, gk_d, w_out, tabk, tabq, M0d, M1d, pen_d,
              nstep=NTO, nbis=24):
    S = C.S
    bank = C.bank
    NTK = 2 * nstep
    LPK = NTK * 128
    W = C.sb([128, 8, 2696], BF16, "aW")
    Wo = C.sb([128, 8, D], BF16, "aWo")
    load_weight_bf16(C, W, w_in, 2696, "aW")
    load_weight_bf16(C, Wo, w_out, D, "aWo")
    g_bc = C.sb([128, D], F32, "a_gbc")
    gq_bc = C.sb([128, 64], F32, "a_gq")
    gk_bc = C.sb([128, 64], F32, "a_gk")
    pen = C.sb([128, 256], F32, "a_pen")
    S.add("sp", lambda e: e.dma_start(out=g_bc[:], in_=g_dram.partition_broadcast(128)), w=[g_bc], dma=g_bc)
    S.add("sp", lambda e: e.dma_start(out=gq_bc[:], in_=gq_d.partition_broadcast(128)), w=[gq_bc], dma=gq_bc)
    S.add("sp", lambda e: e.dma_start(out=gk_bc[:], in_=gk_d.partition_broadcast(128)), w=[gk_bc], dma=gk_bc)
    S.add("sp", lambda e: e.dma_start(out=pen[:], in_=pen_d), w=[pen], dma=pen)
    scr = norm_scratch(C)
    mq = C.sb([128, 1], F32, "a_mq")
    mk_ = C.sb([128, 1], F32, "a_mk")
    negM = C.sb([128, 1], F32, "a_negM")
    S.add("dve", lambda e: e.tensor_reduce(mq[:], gq_bc[:], AX.X, ALU.max, apply_absolute_value=True),
          r=[gq_bc], w=[mq])
    S.add("dve", lambda e: e.tensor_reduce(mk_[:], gk_bc[:], AX.X, ALU.max, apply_absolute_value=True),
          r=[gk_bc], w=[mk_])
    S.add("dve", lambda e: e.tensor_tensor(negM[:], mq[:], mk_[:], ALU.mult), r=[mq, mk_], w=[negM])
    S.add("dve", lambda e: e.tensor_scalar(negM[:], negM[:], -8.0, None, ALU.mult), r=[negM], w=[negM])
    kT_all = C.sb([64, 4, LPK], BF16, "a_kT")
    vext = C.sb([128, NTK, 4, 65], BF16, "a_vext")
    ikT = C.sb([128, LPK], BF16, "a_ikT")
    S.add("pool", lambda e: e.memset(vext[:, :, :, 64:65], 1.0), w=[(vext, "one")])
    score = C.sb([128, LPK], F32, "a_score")
    junk = C.sb([128, LPK], BF16, "a_junk")
    ck64 = C.sb([128, 2, 32], F32, "a_ck64")
    sk64 = C.sb([128, 2, 32], F32, "a_sk64")
    ck128 = C.sb([128, 2, 64], F32, "a_ck128")
    sk128 = C.sb([128, 2, 64], F32, "a_sk128")
    cq64 = C.sb([128, 32], F32, "a_cq64")
    sq64 = C.sb([128, 32], F32, "a_sq64")
    cq128 = C.sb([128, 64], F32, "a_cq128")
    sq128 = C.sb([128, 64], F32, "a_sq128")
    hk = C.sb([128, D], F32, "a_hk")
    hnbk = C.sb([128, D], BF16, "a_hnbk")
    hnTk = C.sb([128, 8, 128], BF16, "a_hnTk")
    sqk = C.sb([128, 256], F32, "a_sqk")
    ssk = C.sb([128, 4], F32, "a_ssk")
    srk = C.sb([128, 4], F32, "a_srk")
    rk = C.sb([128, 4], F32, "a_rk")
    kn = C.sb([128, 256], F32, "a_kn")
    krot = C.sb([128, 256], BF16, "a_krot")
    ikrot = C.sb([128, 128], BF16, "a_ikrot")
    ta = C.sb([128, 512], F32, "a_ta")
    tb = C.sb([128, 512], F32, "a_tb")
    hq = C.sb([128, D], F32, "a_hq")
    hnbq = C.sb([128, D], BF16, "a_hnbq")
    hnTq = C.sb([128, 8, 128], BF16, "a_hnTq")
    ssq = C.sb([128, 16], F32, "a_ssq")
    srq = C.sb([128, 16], F32, "a_srq")
    rq = C.sb([128, 16], F32, "a_rq")
    qn = C.sb([128, D], F32, "a_qn")
    sqq = qn
    qrot = C.sb([128, D], BF16, "a_qrot")
    iqrot = hnbq
    qT = C.sb([64, 16, 128], BF16, "a_qT")
    iqT = C.sb([128, 8, 128], BF16, "a_iqT")
    iw = C.sb([128, 8], F32, "a_iw")
    rl = [C.sb([128, 512], F32, "a_rl%d" % i) for i in range(2)]
    amax = C.sb([128, 1], F32, "a_amax")
    w0 = C.sb([128, 1], F32, "a_w0")
    lo = C.sb([128, 1], F32, "a_lo")
    mid = C.sb([128, 1], F32, "a_mid")
    halfs = C.sb([128, nbis], F32, "a_halfs")
    cnt = C.sb([128, 1], F32, "a_cnt")
    stp = C.sb([128, 1], F32, "a_stp")
    dg = C.sb([128, 128], F32, "a_dg")
    thrbc = C.sb([128, 128], F32, "a_thrbc")
    mkt = [C.sb([128, 128], BF16, "a_mk%d" % i) for i in range(2)]
    pt = [C.sb([128, 4, 128], BF16, "a_pt%d" % i) for i in range(2)]
    ptm = [C.sb([128, 4, 128], BF16, "a_ptm%d" % i) for i in range(2)]
    rd = C.sb([128, 16], F32, "a_rd")
    accs = C.sb([128, 16 * 65], F32, "a_accs")
    attn = qrot
    attnT = hnTq
    pbv0 = bank[0][:].bitcast(BF16)
    tk = [t for t in tabk]
    tq = [t for t in tabq]

    def bc(ap, h):
        return ap.unsqueeze(1).broadcast_to([128, h, ap.shape[-1]])

    for j in range(nstep):
        r0 = 2 * j * 128
        for dst, src in ((ck64, tk[0]), (sk64, tk[1]), (ck128, tk[2]), (sk128, tk[3])):
            S.add("sp", lambda e, dst=dst, src=src, r0=r0: e.dma_start(
                out=dst[:], in_=src[r0:r0 + 256, :].rearrange("(x p) d -> p x d", p=128)), w=[dst], dma=dst)
        for x in range(2):
            ti = 2 * j + x
            load_norm_T(C, h_full(ti), ("hfull", ti), g_bc, scr, hk, hnbk, hnTk,
                        bank[0], hk)
            for kc in range(8):
                mm(C, bank[1][:, 0:512], hnTk[:, kc, :], W[:, kc, 1024:1536], kc == 0, kc == 7, [hnTk, W], [bank[1]])
            for kc in range(8):
                mm(C, bank[4][:, 0:128], hnTk[:, kc, :], W[:, kc, 2560:2688], kc == 0, kc == 7, [hnTk, W], [bank[4]])
            S.add("act", lambda e, ti=ti: e.activation(
                vext[:, ti, :, 0:64], bank[1][:, 256:512].rearrange("p (a b) -> p a b", a=4), AF.Copy),
                r=[bank[1]], w=[(vext, ti)])
            S.add("act", lambda e: e.activation(sqk[:], bank[1][:, 0:256], AF.Square), r=[bank[1]], w=[sqk])
            S.add("dve", lambda e: e.tensor_reduce(ssk[:], sqk[:].rearrange("p (a b) -> p a b", a=4), AX.X, ALU.add),
                  r=[sqk], w=[ssk])
            S.add("act", lambda e: e.activation(srk[:], ssk[:], AF.Sqrt, bias=scr["eps"][:], scale=1.0 / 64.0),
                  r=[ssk, scr["eps"]], w=[srk])
            S.add("dve", lambda e: e.reciprocal(rk[:], srk[:]), r=[srk], w=[rk])
            kn3 = kn[:].rearrange("p (a b) -> p a b", a=4)
            S.add("dve", lambda e, kn3=kn3: e.tensor_tensor(
                kn3, bank[1][:, 0:256].rearrange("p (a b) -> p a b", a=4),
                rk[:].unsqueeze(2).broadcast_to([128, 4, 64]), ALU.mult), r=[bank[1], rk], w=[kn])
            S.add("dve", lambda e, kn3=kn3: e.tensor_tensor(kn3, kn3, bc(gk_bc[:], 4), ALU.mult),
                  r=[kn, gk_bc], w=[kn])
            kr3 = krot[:].rearrange("p (a b) -> p a b", a=4)
            ta3 = ta[:, 0:128].rearrange("p (a b) -> p a b", a=4)
            tb3 = tb[:, 0:128].rearrange("p (a b) -> p a b", a=4)
            rope_ops(C, kn3[:, :, 0:32], kn3[:, :, 32:64], bc(ck64[:, x, :], 4), bc(sk64[:, x, :], 4),
                     kr3[:, :, 0:32], kr3[:, :, 32:64], ta3, tb3, [kn, ck64, sk64], krot)
            for h in range(4):
                S.add("pe", lambda e, h=h: e.transpose(pbv0[0:64, h * 128:(h + 1) * 128],
                                                       krot[:, h * 64:(h + 1) * 64], C.identb[:]),
                      r=[krot, C.identb], w=[bank[0]])
            S.add("act", lambda e, ti=ti: e.activation(
                kT_all[:, :, ti * 128:(ti + 1) * 128], pbv0[0:64, 0:512].rearrange("p (a b) -> p a b", a=4),
                AF.Copy), r=[bank[0]], w=[(kT_all, ti)])
            rope_ops(C, bank[4][:, 0:64], bank[4][:, 64:128], ck128[:, x, :], sk128[:, x, :],
                     ikrot[:, 0:64], ikrot[:, 64:128], ta[:, 0:64], tb[:, 0:64], [bank[4], ck128, sk128], ikrot)
            S.add("pe", lambda e: e.transpose(pbv0[:, 0:128], ikrot[:], C.identb[:]),
                  r=[ikrot, C.identb], w=[bank[0]])
            S.add("act", lambda e, ti=ti: e.activation(ikT[:, ti * 128:(ti + 1) * 128], pbv0[:, 0:128], AF.Copy),
                  r=[bank[0]], w=[(ikT, ti)])
        q0 = j * 128
        for dst, src in ((cq64, tq[0]), (sq64, tq[1]), (cq128, tq[2]), (sq128, tq[3])):
            S.add("sp", lambda e, dst=dst, src=src, q0=q0: e.dma_start(out=dst[:], in_=src[q0:q0 + 128, :]),
                  w=[dst], dma=dst)
        load_norm_T(C, h_own[q0:q0 + 128, :], ("hown", j), g_bc, scr, hq, hnbq, hnTq, bank[0], hq)
        for half in range(2):
            for kc in range(8):
                mm(C, bank[2 + half][:, 0:512], hnTq[:, kc, :], W[:, kc, half * 512:(half + 1) * 512],
                   kc == 0, kc == 7, [hnTq, W], [bank[2 + half]])
            S.add("act", lambda e, half=half: e.activation(sqq[:, half * 512:(half + 1) * 512],
                                                           bank[2 + half][:, 0:512], AF.Square),
                  r=[bank[2 + half]], w=[(sqq, half)])
        S.add("dve", lambda e: e.tensor_reduce(ssq[:], sqq[:].rearrange("p (a b) -> p a b", a=16), AX.X, ALU.add),
              r=[sqq], w=[ssq])
        S.add("act", lambda e: e.activation(srq[:], ssq[:], AF.Sqrt, bias=scr["eps"][:], scale=1.0 / 64.0),
              r=[ssq, scr["eps"]], w=[srq])
        S.add("dve", lambda e: e.reciprocal(rq[:], srq[:]), r=[srq], w=[rq])
        qn3 = qn[:].rearrange("p (a b) -> p a b", a=16)
        for half in range(2):
            S.add("dve", lambda e, half=half: e.tensor_tensor(
                qn3[:, half * 8:(half + 1) * 8, :], bank[2 + half][:, 0:512].rearrange("p (a b) -> p a b", a=8),
                rq[:, half * 8:(half + 1) * 8].unsqueeze(2).broadcast_to([128, 8, 64]), ALU.mult),
                r=[bank[2 + half], rq], w=[(qn, half)])
        S.add("dve", lambda e: e.tensor_tensor(qn3, qn3, bc(gq_bc[:], 16), ALU.mult), r=[qn, gq_bc], w=[qn])
        qr3 = qrot[:].rearrange("p (a b) -> p a b", a=16)
        ta16 = ta[:, 0:512].rearrange("p (a b) -> p a b", a=16)
        tb16 = tb[:, 0:512].rearrange("p (a b) -> p a b", a=16)
        rope_ops(C, qn3[:, :, 0:32], qn3[:, :, 32:64], bc(cq64[:], 16), bc(sq64[:], 16),
                 qr3[:, :, 0:32], qr3[:, :, 32:64], ta16, tb16, [qn, cq64, sq64], qrot)
        for hb in range(2):
            pbv = bank[hb][:].bitcast(BF16)
            for h in range(8):
                hh = hb * 8 + h
                S.add("pe", lambda e, pbv=pbv, h=h, hh=hh: e.transpose(
                    pbv[0:64, h * 128:(h + 1) * 128], qrot[:, hh * 64:(hh + 1) * 64], C.identb[:]),
                    r=[qrot, C.identb], w=[bank[hb]])
            S.add("act", lambda e, pbv=pbv, hb=hb: e.activation(
                qT[:, hb * 8:(hb + 1) * 8, :], pbv[0:64, 0:1024].rearrange("p (a b) -> p a b", a=8), AF.Copy),
                r=[bank[hb]], w=[(qT, hb)])
        for half in range(2):
            for kc in range(8):
                mm(C, bank[2 + half][:, 0:512], hnTq[:, kc, :], W[:, kc, 1536 + half * 512:1536 + (half + 1) * 512],
                   kc == 0, kc == 7, [hnTq, W], [bank[2 + half]])
        for kc in range(8):
            mm(C, bank[4][:, 0:8], hnTq[:, kc, :], W[:, kc, 2688:2696], kc == 0, kc == 7, [hnTq, W], [bank[4]])
        S.add("dve", lambda e: e.tensor_scalar(iw[:], bank[4][:, 0:8], 1.0 / 32.0, None, ALU.mult),
              r=[bank[4]], w=[iw])
        iq3r = iqrot[:].rearrange("p (a b) -> p a b", a=8)
        for half in range(2):
            src3 = bank[2 + half][:, 0:512].rearrange("p (a b) -> p a b", a=4)
            o3 = iq3r[:, half * 4:(half + 1) * 4, :]
            ta4 = ta[:, 0:256].rearrange("p (a b) -> p a b", a=4)
            tb4 = tb[:, 0:256].rearrange("p (a b) -> p a b", a=4)
            rope_ops(C, src3[:, :, 0:64], src3[:, :, 64:128], bc(cq128[:], 4), bc(sq128[:], 4),
                     o3[:, :, 0:64], o3[:, :, 64:128], ta4, tb4, [bank[2 + half], cq128, sq128], iqrot, wkey=half)
        transpose_to(C, iqrot, iqT, 8, bank[0])
        NK = (2 * j + 2) * 128
        k0 = 0
        blk = 0
        while k0 < NK:
            kn_ = min(512, NK - k0)
            for h in range(8):
                pb = bank[4 + blk % 2]
                rb = rl[blk % 2]
                blk += 1
                mm(C, pb[:, 0:kn_], iqT[:, h, :], ikT[:, k0:k0 + kn_], True, True, [iqT, ikT], [pb])
                S.add("act", lambda e, pb=pb, rb=rb, kn_=kn_: e.activation(rb[:, 0:kn_], pb[:, 0:kn_], AF.Relu),
                      r=[pb], w=[rb])
                if h == 0:
                    S.add("dve", lambda e, rb=rb, k0=k0, kn_=kn_: e.tensor_scalar(
                        score[:, k0:k0 + kn_], rb[:, 0:kn_], iw[:, 0:1], None, ALU.mult),
                        r=[rb, iw], w=[(score, k0)])
                else:
                    S.add("dve", lambda e, rb=rb, k0=k0, kn_=kn_, h=h: e.scalar_tensor_tensor(
                        score[:, k0:k0 + kn_], rb[:, 0:kn_], iw[:, h:h + 1], score[:, k0:k0 + kn_],
                        ALU.mult, ALU.add), r=[rb, iw, (score, k0)], w=[(score, k0)])
            k0 += kn_
        S.add("dve", lambda e, NK=NK: e.tensor_reduce(amax[:], score[:, 0:NK], AX.X, ALU.max,
                                                      apply_absolute_value=True), r=[score], w=[amax])
        S.add("dve", lambda e, NK=NK: e.tensor_tensor(score[:, NK - 256:NK], score[:, NK - 256:NK], pen[:], ALU.add),
              r=[score, pen], w=[score])
        S.add("dve", lambda e: e.tensor_scalar(lo[:], amax[:], -1.0, -1.0, ALU.mult, ALU.add), r=[amax], w=[lo])
        S.add("dve", lambda e: e.tensor_scalar(w0[:], amax[:], 2.0, 2.0, ALU.mult, ALU.add), r=[amax], w=[w0])
        for it in range(nbis):
            S.add("dve", lambda e, it=it: e.tensor_scalar(halfs[:, it:it + 1], w0[:], float(2.0 ** -(it + 1)), None,
                                                          ALU.mult), r=[w0], w=[(halfs, it)])
        for it in range(nbis):
            S.add("dve", lambda e, it=it: e.tensor_tensor(mid[:], lo[:], halfs[:, it:it + 1], ALU.add),
                  r=[lo, (halfs, it)], w=[mid])
            S.add("dve", lambda e, NK=NK: e.tensor_scalar(junk[:, 0:NK], score[:, 0:NK], mid[:, 0:1], 0.0,
                                                          ALU.is_ge, ALU.add, accum_out=cnt[:]),
                  r=[score, mid], w=[junk, cnt])
            S.add("dve", lambda e, it=it: e.scalar_tensor_tensor(stp[:], cnt[:], 256.0, halfs[:, it:it + 1],
                                                                 ALU.is_ge, ALU.mult),
                  r=[cnt, (halfs, it)], w=[stp])
            S.add("dve", lambda e: e.tensor_tensor(lo[:], lo[:], stp[:], ALU.add), r=[lo, stp], w=[lo])
        S.add("dve", lambda e: e.tensor_scalar(dg[:], C.ident32[:], lo[:, 0:1], None, ALU.mult),
              r=[C.ident32, lo], w=[dg])
        mm(C, bank[0][:, 0:128], C.ones32[:], dg[:], True, True, [C.ones32, dg], [bank[0]])
        S.add("dve", lambda e: e.tensor_copy(thrbc[:], bank[0][:, 0:128]), r=[bank[0]], w=[thrbc])
        if getattr(C, "dbg", None) and j == C.dbg["j"]:
            dd_ = C.dbg
            S.add("sp", lambda e: e.dma_start(out=dd_["score"], in_=score[:, 0:dd_["score"].shape[1]]), r=[score],
                  w=["dbg_score"], dma=score)
            S.add("sp", lambda e: e.dma_start(out=dd_["lo"], in_=lo[:]), r=[lo], w=["dbg_lo"], dma=lo)
            S.add("sp", lambda e: e.dma_start(out=dd_["thrbc"], in_=thrbc[:]), r=[thrbc], w=["dbg_thr"], dma=thrbc)
            S.add("sp", lambda e: e.dma_start(out=dd_["amax"], in_=amax[:]), r=[amax], w=["dbg_amax"], dma=amax)
            S.add("sp", lambda e: e.dma_start(out=dd_["cnt"], in_=cnt[:]), r=[cnt], w=["dbg_cnt"], dma=cnt)
        nkt = 2 * j + 2
        accb = [bank[1], bank[2], bank[3]]
        ub = 0
        for kt in range(nkt):
            mkb = mkt[kt % 2]
            S.add("pe", lambda e, kt=kt: e.transpose(bank[0][:, 128:256], score[:, kt * 128:(kt + 1) * 128],
                                                     C.ident32[:]), r=[score, C.ident32], w=[bank[0]])
            S.add("dve", lambda e, mkb=mkb: e.tensor_tensor(mkb[:], bank[0][:, 128:256], thrbc[:], ALU.is_ge),
                  r=[bank[0], thrbc], w=[mkb])
            for g in range(4):
                sb_ = bank[6 + ub % 2]
                ptb, ptmb = pt[ub % 2], ptm[ub % 2]
                ub += 1
                mm(C, sb_[:, 0:512], kT_all[:, g, kt * 128:(kt + 1) * 128],
                   qT[:, 4 * g:4 * g + 4, :].rearrange("p a b -> p (a b)"), True, True, [kT_all, qT], [sb_])
                S.add("act", lambda e, sb_=sb_, ptb=ptb: e.activation(
                    ptb[:].rearrange("p a b -> p (a b)"), sb_[:, 0:512], AF.Exp, bias=negM[:], scale=0.125),
                    r=[sb_, negM], w=[ptb])
                S.add("dve", lambda e, ptb=ptb, ptmb=ptmb, mkb=mkb: e.tensor_tensor(
                    ptmb[:], ptb[:], bc(mkb[:], 4), ALU.mult), r=[ptb, mkb], w=[ptmb])
                for hh in range(4):
                    hd = 4 * g + hh
                    ab = accb[hd // 7]
                    c0 = (hd % 7) * 65
                    mm(C, ab[:, c0:c0 + 65], ptmb[:, hh, :], vext[:, kt, g, :], True, True,
                       [ptmb, vext], [(ab, hd)])
            for bi, (h0, nh) in enumerate(((0, 7), (7, 7), (14, 2))):
                if kt == 0:
                    S.add("dve", lambda e, bi=bi, h0=h0, nh=nh: e.tensor_copy(
                        accs[:, h0 * 65:(h0 + nh) * 65], accb[bi][:, 0:nh * 65]), r=[accb[bi]], w=[(accs, bi)])
                else:
                    S.add("dve", lambda e, bi=bi, h0=h0, nh=nh: e.tensor_tensor(
                        accs[:, h0 * 65:(h0 + nh) * 65], accb[bi][:, 0:nh * 65], accs[:, h0 * 65:(h0 + nh) * 65],
                        ALU.add), r=[accb[bi], (accs, bi)], w=[(accs, bi)])
        at3 = attn[:].rearrange("p (a b) -> p a b", a=16)
        for bi, (h0, nh) in enumerate(((0, 7), (7, 7), (14, 2))):
            a3 = accs[:, h0 * 65:(h0 + nh) * 65].rearrange("p (a b) -> p a b", a=nh)
            S.add("dve", lambda e, a3=a3, h0=h0, nh=nh: e.reciprocal(rd[:, h0:h0 + nh], a3[:, :, 64]),
                  r=[(accs, bi)], w=[(rd, bi)])
            S.add("dve", lambda e, a3=a3, h0=h0, nh=nh: e.tensor_tensor(
                at3[:, h0:h0 + nh, :], a3[:, :, 0:64], rd[:, h0:h0 + nh].unsqueeze(2).broadcast_to([128, nh, 64]),
                ALU.mult), r=[(accs, bi), (rd, bi)], w=[(attn, bi)])
        transpose_to(C, attn, attnT, 8, bank[0])
        for half in range(2):
            for kc in range(8):
                mm(C, bank[4 + half][:, 0:512], attnT[:, kc, :], Wo[:, kc, half * 512:(half + 1) * 512],
                   kc == 0, kc == 7, [attnT, Wo], [bank[4 + half]])
            S.add("dve", lambda e, half=half: e.tensor_tensor(hq[:, half * 512:(half + 1) * 512],
                                                              bank[4 + half][:, 0:512],
                                                              hq[:, half * 512:(half + 1) * 512], ALU.add),
                  r=[bank[4 + half], hq], w=[hq])
        S.add("sp", lambda e, j=j: e.dma_start(out=out_dst[j * 128:(j + 1) * 128, :], in_=hq[:]),
              r=[hq], w=[("aout", j)], dma=hq)


NCORES = 8
_PROG = None
_PLAN = None
GROUPS = [[0, 1], [2, 3], [4, 5], [6, 7]]


def _rope_tabs(pos, half):
    inv = (10000.0 ** (-np.arange(half, dtype=np.float32) / np.float32(half))).astype(np.float32)
    ang = (pos.astype(np.float32)[:, None] * inv[None, :]).astype(np.float32)
    return np.cos(ang).astype(np.float32), np.sin(ang).astype(np.float32)


def _dt(nc, n, s, k="ExternalInput"):
    return nc.dram_tensor(n, list(s), F32, kind=k).ap()


def _build_fused(plan=None):
    if plan is None:
        plan = [("dsa", 0), ("ffn", 0), ("mlstm", 1), ("moe", 1), ("dsa", 2), ("ffn", 2), ("mlstm", 3), ("moe", 3)]
    nc = bass.Bass("TRN2", target_bir_lowering=False)
    NR = NTO * 128
    decl = {}

    def inp(name, shape):
        if name not in decl:
            decl[name] = _dt(nc, name, shape)
        return decl[name]

    x0 = inp("x0", [NR, D])
    out = _dt(nc, "out", [NR, D], "ExternalOutput")
    xo_t = nc.dram_tensor("xo", [NR, D], F32)
    xa_t = nc.dram_tensor("xa", [2 * NR, D], F32)
    xo, xa = xo_t.ap(), xa_t.ap()
    xm = xo

    def hf_tile(ti):
        return xa[ti * 128:(ti + 1) * 128, :]

    def gather(C):
        def after(out_key):
            for jj in range(NTO):
                C.S.add("pool", lambda e, jj=jj: e.collective_compute(
                    "AllGather", ALU.bypass, replica_groups=GROUPS,
                    ins=[xo_t.ap()[jj * 128:(jj + 1) * 128, :].opt()],
                    outs=[xa_t.ap()[2 * jj * 128:2 * (jj + 1) * 128, :].opt()]),
                    r=[(out_key, jj)], w=[("xa", jj)], dma="cc", cc=True)
        return after

    def phase(fn):
        with nc.cleanup_on_exit():
            with ExitStack() as es:
                C = Ctx(nc, es)
                C.consts()
                fn(C)
                C.S.emit()

    def p0(C):
        for jj in range(NTO):
            C.S.add("sp", lambda e, jj=jj: e.dma_start(out=xo[jj * 128:(jj + 1) * 128, :],
                                                       in_=x0[jj * 128:(jj + 1) * 128, :]),
                    w=[("xo", jj)], dma=("cp0", jj))
        gather(C)("xo")
    phase(p0)
    for pi, (kind, i) in enumerate(plan):
        j = i // 2
        last = pi == len(plan) - 1
        if kind in ("dsa", "mlstm"):
            M0 = inp("M0", [128, 128]); M1 = inp("M1", [128, 128])
            nm = inp("norm_mixer", [4, D])
            dst = out if last else xm
        if kind in ("ffn", "moe"):
            nf = inp("norm_ffn", [4, D]); rowmask = inp("rowmask", [128, 1])
            dst = out if last else xo
            src = xm if (pi > 0 and plan[pi - 1][0] in ("dsa", "mlstm")) else xo
        if kind == "dsa":
            a_win = inp("dsa_w_in", [2, D, 2696]); a_gq = inp("dsa_q_norm", [2, 64])
            a_gk = inp("dsa_k_norm", [2, 64]); a_wo = inp("dsa_w_out", [2, D, D]); pen = inp("pen", [128, 256])
            tabk = [inp("tk%d" % t, [LP, 32 if t < 2 else 64]) for t in range(4)]
            tabq = [inp("tq%d" % t, [NR, 32 if t < 2 else 64]) for t in range(4)]
            phase(lambda C: dsa_phase(C, hf_tile, xo, dst, nm[i], a_win[j], a_gq[j], a_gk[j], a_wo[j],
                                      tabk, tabq, M0, M1, pen))
        elif kind == "mlstm":
            m_win = inp("mlstm_w_in", [2, D, 3080]); m_bi = inp("mlstm_b_i", [2, 4])
            m_bf = inp("mlstm_b_f", [2, 4]); m_go = inp("mlstm_out_norm", [2, D]); m_wo = inp("mlstm_w_out", [2, D, D])
            phase(lambda C: mlstm_phase(C, hf_tile, xo, dst, nm[i], m_win[j], m_bi[j], m_bf[j], m_go[j],
                                        m_wo[j], M0, M1))
        elif kind == "ffn":
            f_wg = inp("ffn_w_gate", [2, D, DFF]); f_wu = inp("ffn_w_up", [2, D, DFF]); f_wd = inp("ffn_w_down", [2, DFF, D])
            phase(lambda C: ffn_phase(C, src, dst, nf[i], [(f_wg[j], f_wu[j], f_wd[j])], None,
                                      out_key="xo", rowmask=rowmask, after=None if last else gather(C)))
        elif kind == "moe":
            e_rt = inp("moe_router", [2, D, NE]); e_wg = inp("moe_w_gate", [2, NE, D, DFF])
            e_wu = inp("moe_w_up", [2, NE, D, DFF]); e_wd = inp("moe_w_down", [2, NE, DFF, D])
            phase(lambda C: ffn_phase(C, src, dst, nf[i],
                                      [(e_wg[j][e], e_wu[j][e], e_wd[j][e]) for e in range(NE)], e_rt[j],
                                      out_key="xo", rowmask=rowmask, after=None if last else gather(C)))
    nc._decl_inputs = set(decl)
    return nc


def _own_rows(r):
    return np.concatenate([np.arange((2 * j + r) * 128, (2 * j + r + 1) * 128) for j in range(NTO)])


def kernel(x, meta, norm_mixer, norm_ffn, dsa_w_in, dsa_q_norm, dsa_k_norm, dsa_w_out,
           mlstm_w_in, mlstm_b_i, mlstm_b_f, mlstm_out_norm, mlstm_w_out,
           ffn_w_gate, ffn_w_up, ffn_w_down, moe_router, moe_w_gate, moe_w_up, moe_w_down):
    global _PROG
    f = lambda a: np.ascontiguousarray(np.asarray(a), dtype=np.float32)
    x, meta = f(x), f(meta)
    B = x.shape[0]
    h = np.zeros((B, LP, D), np.float32)
    h[:, :NMETA] = meta[None]
    h[:, NMETA:LTOK] = x
    pos = np.arange(LP)
    c64, s64 = _rope_tabs(pos, 32)
    c128, s128 = _rope_tabs(pos, 64)
    s_ = np.arange(128)[:, None]
    t_ = np.arange(128)[None, :]
    shared = dict(norm_mixer=f(norm_mixer), norm_ffn=f(norm_ffn), dsa_w_in=f(dsa_w_in), dsa_q_norm=f(dsa_q_norm),
                  dsa_k_norm=f(dsa_k_norm), dsa_w_out=f(dsa_w_out), mlstm_w_in=f(mlstm_w_in),
                  mlstm_b_i=f(mlstm_b_i), mlstm_b_f=f(mlstm_b_f), mlstm_out_norm=f(mlstm_out_norm),
                  mlstm_w_out=f(mlstm_w_out), ffn_w_gate=f(ffn_w_gate), ffn_w_up=f(ffn_w_up),
                  ffn_w_down=f(ffn_w_down), moe_router=f(moe_router), moe_w_gate=f(moe_w_gate),
                  moe_w_up=f(moe_w_up), moe_w_down=f(moe_w_down), tk0=c64, tk1=s64, tk2=c128, tk3=s128)
    per_r = []
    for r in range(2):
        M0 = (s_ <= t_ + 128 * r).astype(np.float32)
        M1 = (128 + s_ <= t_ + 128 * r).astype(np.float32)
        pen = np.ascontiguousarray(np.where(np.concatenate([M0, M1], 0).T > 0, 0.0, -3e38).astype(np.float32))
        rows = _own_rows(r)
        rowmask = (rows[-128:] < LTOK).astype(np.float32)[:, None]
        per_r.append(dict(M0=M0, M1=M1, pen=pen, rowmask=np.ascontiguousarray(rowmask),
                          tq0=np.ascontiguousarray(c64[rows]), tq1=np.ascontiguousarray(s64[rows]),
                          tq2=np.ascontiguousarray(c128[rows]), tq3=np.ascontiguousarray(s128[rows]), rows=rows))
    maps = []
    for c in range(NCORES):
        b, r = c // 2, c % 2
        m = dict(shared)
        m.update({k: v for k, v in per_r[r].items() if k != "rows"})
        m["x0"] = np.ascontiguousarray(h[b][per_r[r]["rows"]])
        maps.append(m)
    if _PROG is None:
        _PROG = _build_fused(_PLAN)
    maps = [{k: v for k, v in m.items() if k in _PROG._decl_inputs} for m in maps]
    res = run_bass_kernel_spmd(_PROG, maps, core_ids=list(range(NCORES)))
    hn = np.empty_like(h)
    for c in range(NCORES):
        hn[c // 2][per_r[c % 2]["rows"]] = res.results[c]["out"]
    return np.ascontiguousarray(hn[:, NMETA:LTOK])
```

```python
import numpy as np
from contextlib import ExitStack
import concourse.bass as bass
import concourse.mybir as mybir
from concourse.bass_utils import run_bass_kernel_spmd

F32 = mybir.dt.float32
BF16 = mybir.dt.bfloat16
I32 = mybir.dt.int32
AF = mybir.ActivationFunctionType
ALU = mybir.AluOpType
AX = mybir.AxisListType

D = 1024
NMETA = 16
SEQ = 4096
LTOK = SEQ + NMETA
NT = 34
LP = NT * 128
NTO = 17
DFF = 3584
NE = 8
EPS = 1e-6


class Sched:
    ENG = ("pe", "act", "dve", "pool", "sp")
    NPH = 0

    def __init__(self, nc, es, same_engine_sync=True):
        self.nc = nc
        self.es = es
        self.ops = []
        self.state = {}
        self.same = same_engine_sync
        self.dma_keys = {}

    @staticmethod
    def _norm(res):
        if isinstance(res, tuple):
            return id(res[0]) if not isinstance(res[0], str) else res[0], res[1]
        return (id(res) if not isinstance(res, str) else res), None

    def _conf(self, t, k):
        st = self.state.get(t)
        if not st:
            return []
        if k is None:
            return list(st.values())
        out = []
        if None in st:
            out.append(st[None])
        if k in st:
            out.append(st[k])
        return out

    def add(self, eng, fn, r=(), w=(), dma=None, cc=False):
        idx = len(self.ops)
        deps = set()
        rn = [self._norm(x) for x in r]
        wn = [self._norm(x) for x in w]
        for t, k in rn:
            for e in self._conf(t, k):
                if e[0] is not None:
                    deps.add(e[0])
        for t, k in wn:
            for e in self._conf(t, k):
                if e[0] is not None:
                    deps.add(e[0])
                deps.update(e[1])
        for t, k in rn:
            st = self.state.setdefault(t, {})
            st.setdefault(k, [None, []])[1].append(idx)
        for t, k in wn:
            st = self.state.setdefault(t, {})
            if k is None:
                st.clear()
            st[k] = [idx, []]
        deps.discard(idx)
        if dma is not None:
            dma = self._norm(dma)
        self.ops.append(dict(eng=eng, fn=fn, deps=deps, dma=dma, inc=False, count=None, cc=cc))
        return idx

    def _skip(self, dep, op):
        if dep["dma"] is not None:
            return False
        if dep["eng"] == op["eng"]:
            if dep["eng"] == "pe" and op["dma"] is None:
                return True
            if not self.same:
                return True
        return False

    def emit(self):
        nc, es, ops = self.nc, self.es, self.ops
        for op in ops:
            for d in op["deps"]:
                dep = ops[d]
                if dep["dma"] is None and not self._skip(dep, op):
                    dep["inc"] = True
        cnt = {e: 0 for e in self.ENG}
        dcnt = {}
        for op in ops:
            if op["dma"] is not None:
                dcnt[op["dma"]] = dcnt.get(op["dma"], 0) + (1 if op["cc"] else 16)
                op["count"] = dcnt[op["dma"]]
            elif op["inc"]:
                cnt[op["eng"]] += 1
                op["count"] = cnt[op["eng"]]
        Sched.NPH += 1
        pfx = "p%d_" % Sched.NPH
        sems = {e: nc.alloc_semaphore(name=pfx + "s_" + e) for e in self.ENG}
        dsem = {}
        for i, k in enumerate(dcnt):
            dsem[k] = nc.alloc_semaphore(name=pfx + "d%d" % i)
        self.n_sems = len(sems) + len(dsem)

        def run(engname, e):
            waited = {}
            for op in ops:
                if op["eng"] != engname:
                    continue
                need = {}
                for d in op["deps"]:
                    dep = ops[d]
                    if self._skip(dep, op):
                        continue
                    if dep["dma"] is not None:
                        key, sem = ("d", dep["dma"]), dsem[dep["dma"]]
                    else:
                        key, sem = ("e", dep["eng"]), sems[dep["eng"]]
                    if need.get(key, (None, 0))[1] < dep["count"]:
                        need[key] = (sem, dep["count"])
                for key, (sem, val) in need.items():
                    if waited.get(key, 0) >= val:
                        continue
                    e.wait_ge(sem, val)
                    waited[key] = val
                ins = op["fn"](e)
                if op["dma"] is not None:
                    if op["cc"]:
                        ins.then_inc(dsem[op["dma"]])
                    else:
                        ins.then_inc(dsem[op["dma"]], 16)
                elif op["inc"]:
                    ins.then_inc(sems[op["eng"]], 1)
            if engname == "sp":
                for k, v in dcnt.items():
                    e.wait_ge(dsem[k], v)

        with nc.Block() as block:
            @block.tensor
            def _(e):
                run("pe", e)

            @block.scalar
            def _(e):
                run("act", e)

            @block.vector
            def _(e):
                run("dve", e)

            @block.gpsimd
            def _(e):
                run("pool", e)

            @block.sync
            def _(e):
                run("sp", e)


class Ctx:
    NCTX = 0

    def __init__(self, nc, es):
        self.nc = nc
        self.es = es
        self.S = Sched(nc, es)
        self.n = 0
        Ctx.NCTX += 1
        self.pfx = "c%d_" % Ctx.NCTX
        self.bank = [es.enter_context(nc.psum_tensor(self.pfx + "bank%d" % i, [128, 512], F32)) for i in range(8)]

    def sb(self, shape, dt, name=None):
        self.n += 1
        return self.es.enter_context(self.nc.sbuf_tensor(self.pfx + (name or ("t%d" % self.n)), list(shape), dt))

    def consts(self):
        S = self.S
        self.ones32 = self.sb([128, 128], F32, "ones32")
        self.ident32 = self.sb([128, 128], F32, "ident32")
        self.identb = self.sb([128, 128], BF16, "identb")
        S.add("pool", lambda e: e.memset(self.ones32[:], 1.0), w=[self.ones32])
        S.add("pool", lambda e: e.affine_select(self.ident32[:], self.ones32[:], [[-1, 128]],
                                                ALU.is_equal, 0.0, base=0, channel_multiplier=1),
              r=[self.ones32], w=[self.ident32])
        S.add("pool", lambda e: e.tensor_copy(self.identb[:], self.ident32[:]),
              r=[self.ident32], w=[self.identb])


def rmsnorm_tile(C, h_ap, h_res, g_bc, outs, scr):
    S = C.S
    junk, ss, sq, rstd = scr["junk"], scr["ss"], scr["sq"], scr["rstd"]
    S.add("act", lambda e: e.activation(junk[:], h_ap, AF.Square, accum_out=ss[:]),
          r=[h_res], w=[junk, ss])
    S.add("act", lambda e: e.activation(sq[:], ss[:], AF.Sqrt, bias=scr["eps"][:], scale=1.0 / D),
          r=[ss, scr["eps"]], w=[sq])
    S.add("dve", lambda e: e.reciprocal(rstd[:], sq[:]), r=[sq], w=[rstd])
    for ap, res in outs:
        S.add("dve", lambda e, ap=ap: e.scalar_tensor_tensor(ap, h_ap, rstd[:, 0:1], g_bc[:],
                                                             ALU.mult, ALU.mult),
              r=[h_res, rstd, g_bc], w=[res])


def norm_scratch(C):
    scr = dict(junk=C.sb([128, D], F32), ss=C.sb([128, 1], F32), sq=C.sb([128, 1], F32),
               rstd=C.sb([128, 1], F32), eps=C.sb([128, 1], F32))
    C.S.add("pool", lambda e: e.memset(scr["eps"][:], EPS), w=[scr["eps"]])
    return scr


def ffn_phase(C, h_src, out_dst, g_dram, experts, router, ntile=NTO, h_key="h", out_key="out",
              rowmask=None, after=None):
    nc, S = C.nc, C.S
    NTOK = ntile * 128
    moe = router is not None
    GC = 2
    NG = DFF // (128 * GC)
    yacc = C.sb([128, ntile, D], F32, "yacc")
    hnT = C.sb([128, 8, NTOK], BF16, "hnT")
    g_bc = C.sb([128, D], F32, "g_bc")
    scr = norm_scratch(C)
    hnb = [C.sb([128, D], BF16, "hnb%d" % i) for i in range(2)]
    S.add("sp", lambda e: e.dma_start(out=g_bc[:], in_=g_dram.partition_broadcast(128)),
          w=[g_bc], dma=g_bc)
    if moe:
        hn32 = [C.sb([128, D], F32, "hn32_%d" % i) for i in range(2)]
        hnT32 = [C.sb([128, 8, 128], F32, "hnT32_%d" % i) for i in range(2)]
        wr = C.sb([128, 8, NE], F32, "wr")
        call = C.sb([128, ntile, NE], F32, "call")
        rt = dict(lg=C.sb([128, NE], F32), m8=C.sb([128, 8], F32), negm=C.sb([128, 1], F32),
                  mask=C.sb([128, NE], F32), ex=C.sb([128, NE], F32), p=C.sb([128, NE], F32),
                  den=C.sb([128, 1], F32), rden=C.sb([128, 1], F32))
        S.add("sp", lambda e: e.dma_start(out=wr[:], in_=router.rearrange("(kc p) n -> p kc n", p=128)),
              w=[wr], dma=wr)

    for ti in range(ntile):
        hb = hnb[ti % 2]
        S.add("sp", lambda e, ti=ti: e.dma_start(out=yacc[:, ti, :], in_=h_src[ti * 128:(ti + 1) * 128, :]),
              r=[(h_key, ti)], w=[(yacc, ti)], dma=(yacc, ti))
        outs = [(hb[:], hb)]
        if moe:
            outs.append((hn32[ti % 2][:], hn32[ti % 2]))
        rmsnorm_tile(C, yacc[:, ti, :], (yacc, ti), g_bc, outs, scr)
        pb = C.bank[4 + 2 * (ti % 2)]
        pbv = pb[:].bitcast(BF16)
        for kc in range(8):
            S.add("pe", lambda e, kc=kc, hb=hb, pbv=pbv: e.transpose(
                pbv[:, kc * 128:(kc + 1) * 128], hb[:, kc * 128:(kc + 1) * 128], C.identb[:]),
                r=[hb, C.identb], w=[pb])
        S.add("act", lambda e, ti=ti, pbv=pbv: e.activation(
            hnT[:, :, ti * 128:(ti + 1) * 128], pbv.rearrange("p (a b) -> p a b", a=8), AF.Copy),
            r=[pb], w=[(hnT, ti)])
        if moe:
            h32, hT32 = hn32[ti % 2], hnT32[ti % 2]
            pa, pbk = C.bank[0 + 2 * (ti % 2)], C.bank[1 + 2 * (ti % 2)]
            for kc in range(8):
                bk = pa if kc < 4 else pbk
                S.add("pe", lambda e, kc=kc, bk=bk, h32=h32: e.transpose(
                    bk[:, (kc % 4) * 128:(kc % 4 + 1) * 128], h32[:, kc * 128:(kc + 1) * 128], C.ident32[:]),
                    r=[h32, C.ident32], w=[bk])
            S.add("dve", lambda e, pa=pa, hT32=hT32: e.tensor_copy(
                hT32[:, 0:4, :], pa[:].rearrange("p (a b) -> p a b", a=4)), r=[pa], w=[(hT32, 0)])
            S.add("dve", lambda e, pbk=pbk, hT32=hT32: e.tensor_copy(
                hT32[:, 4:8, :], pbk[:].rearrange("p (a b) -> p a b", a=4)), r=[pbk], w=[(hT32, 1)])
            lgp = C.bank[4 + 2 * (ti % 2) + 1]
            for kc in range(8):
                S.add("pe", lambda e, kc=kc, hT32=hT32, lgp=lgp: e.matmul(
                    lgp[:, 0:NE], hT32[:, kc, :], wr[:, kc, :], start=(kc == 0), stop=(kc == 7)),
                    r=[hT32, wr], w=[lgp])
            lg, m8, negm, mask, ex, p, den, rden = (rt[k] for k in
                                                    ("lg", "m8", "negm", "mask", "ex", "p", "den", "rden"))
            S.add("dve", lambda e, lgp=lgp: e.tensor_copy(lg[:], lgp[:, 0:NE]), r=[lgp], w=[lg])
            S.add("dve", lambda e: e.max(m8[:], lg[:]), r=[lg], w=[m8])
            S.add("dve", lambda e: e.tensor_scalar(negm[:], m8[:, 0:1], -1.0, None, ALU.mult),
                  r=[m8], w=[negm])
            S.add("dve", lambda e: e.tensor_scalar(mask[:], lg[:], m8[:, 1:2], None, ALU.is_ge),
                  r=[lg, m8], w=[mask])
            S.add("act", lambda e: e.activation(ex[:], lg[:], AF.Exp, bias=negm[:], scale=1.0),
                  r=[lg, negm], w=[ex])
            S.add("dve", lambda e: e.tensor_tensor(p[:], ex[:], mask[:], ALU.mult), r=[ex, mask], w=[p])
            S.add("dve", lambda e: e.tensor_reduce(den[:], p[:], AX.X, ALU.add), r=[p], w=[den])
            S.add("dve", lambda e: e.reciprocal(rden[:], den[:]), r=[den], w=[rden])
            S.add("dve", lambda e, ti=ti: e.tensor_scalar(call[:, ti, :], p[:], rden[:, 0:1], None, ALU.mult),
                  r=[p, rden], w=[(call, ti)])

    NWB = 3
    wgt = [C.sb([128, 8, GC * 128], BF16, "wg%d" % i) for i in range(NWB)]
    wut = [C.sb([128, 8, GC * 128], BF16, "wu%d" % i) for i in range(NWB)]
    wdt = [C.sb([128, GC, D], BF16, "wd%d" % i) for i in range(NWB)]
    actT = [C.sb([128, GC, NTOK], BF16, "actT%d" % i) for i in range(2)]
    sgt = [C.sb([128, 512], F32, "sg%d" % i) for i in range(2)]
    PG = [C.bank[0], C.bank[1]]
    PU = [C.bank[2], C.bank[3]]
    PD = [(C.bank[4], C.bank[5]), (C.bank[6], C.bank[7])]
    tblocks = []
    t0 = 0
    while t0 < NTOK:
        tblocks.append((t0, min(512, NTOK - t0)))
        t0 += 512
    state = dict(blk=0, dcount=0)

    def emit_gateup(wgb, wub, at, ci, tb0, tbn):
        b = state["blk"] % 2
        state["blk"] += 1
        pg, pu, sg = PG[b], PU[b], sgt[b]
        for kc in range(8):
            S.add("pe", lambda e, kc=kc: e.matmul(
                pg[:, 0:tbn], wgb[:, kc, ci * 128:(ci + 1) * 128], hnT[:, kc, tb0:tb0 + tbn],
                start=(kc == 0), stop=(kc == 7)), r=[wgb, hnT], w=[pg])
        for kc in range(8):
            S.add("pe", lambda e, kc=kc: e.matmul(
                pu[:, 0:tbn], wub[:, kc, ci * 128:(ci + 1) * 128], hnT[:, kc, tb0:tb0 + tbn],
                start=(kc == 0), stop=(kc == 7)), r=[wub, hnT], w=[pu])
        S.add("act", lambda e: e.activation(sg[:, 0:tbn], pg[:, 0:tbn], AF.Silu), r=[pg], w=[sg])
        S.add("dve", lambda e: e.tensor_tensor(at[:, ci, tb0:tb0 + tbn], sg[:, 0:tbn], pu[:, 0:tbn], ALU.mult),
              r=[sg, pu], w=[(at, (ci, tb0))])

    def emit_down(ei, wdb, at, ti):
        pd = PD[state["dcount"] % 2]
        state["dcount"] += 1
        for half in range(2):
            for ci in range(GC):
                S.add("pe", lambda e, half=half, ci=ci: e.matmul(
                    pd[half][:, :], at[:, ci, ti * 128:(ti + 1) * 128], wdb[:, ci, half * 512:(half + 1) * 512],
                    start=(ci == 0), stop=(ci == GC - 1)), r=[at, wdb], w=[pd[half]])
        for half in range(2):
            ysl = yacc[:, ti, half * 512:(half + 1) * 512]
            if moe:
                S.add("dve", lambda e, ysl=ysl, half=half: e.scalar_tensor_tensor(
                    ysl, pd[half][:, :], call[:, ti, ei:ei + 1], ysl, ALU.mult, ALU.add),
                    r=[pd[half], (call, ti), (yacc, ti)], w=[(yacc, ti)])
            else:
                S.add("dve", lambda e, ysl=ysl, half=half: e.tensor_tensor(ysl, pd[half][:, :], ysl, ALU.add),
                      r=[pd[half], (yacc, ti)], w=[(yacc, ti)])

    groups_ = [(ei, gi) for ei in range(len(experts)) for gi in range(NG)]
    pending = None
    for gn, (ei, gi) in enumerate(groups_):
        wg, wu, wd = experts[ei]
        c0 = gi * GC * 128
        wb = gn % NWB
        wgb, wub, wdb, at = wgt[wb], wut[wb], wdt[wb], actT[gn % 2]
        S.add("pool", lambda e, wgb=wgb, wg=wg, c0=c0: e.dma_start(
            out=wgb[:], in_=wg[:, c0:c0 + GC * 128].rearrange("(kc p) c -> p kc c", p=128)),
            w=[wgb], dma=wgb)
        S.add("pool", lambda e, wub=wub, wu=wu, c0=c0: e.dma_start(
            out=wub[:], in_=wu[:, c0:c0 + GC * 128].rearrange("(kc p) c -> p kc c", p=128)),
            w=[wub], dma=wub)
        S.add("pool", lambda e, wdb=wdb, wd=wd, c0=c0: e.dma_start(
            out=wdb[:], in_=wd[c0:c0 + GC * 128, :].rearrange("(j p) n -> p j n", p=128)),
            w=[wdb], dma=wdb)
        slot = 0
        for ci in range(GC):
            for (tb0, tbn) in tblocks:
                emit_gateup(wgb, wub, at, ci, tb0, tbn)
                if pending is not None:
                    for ti in (2 * slot, 2 * slot + 1):
                        if ti < ntile:
                            emit_down(pending[0], pending[1], pending[2], ti)
                slot += 1
        if pending is not None:
            for ti in range(2 * slot, ntile):
                emit_down(pending[0], pending[1], pending[2], ti)
        pending = (ei, wdb, at)
    for ti in range(ntile):
        emit_down(pending[0], pending[1], pending[2], ti)
    if rowmask is not None:
        rm = C.sb([128, 1], F32, "rowmask")
        S.add("sp", lambda e: e.dma_start(out=rm[:], in_=rowmask), w=[rm], dma=rm)
        S.add("dve", lambda e: e.tensor_scalar(yacc[:, ntile - 1, :], yacc[:, ntile - 1, :], rm[:, 0:1], None,
                                               ALU.mult), r=[(yacc, ntile - 1), rm], w=[(yacc, ntile - 1)])
    for ti in range(ntile):
        S.add("sp", lambda e, ti=ti: e.dma_start(out=out_dst[ti * 128:(ti + 1) * 128, :], in_=yacc[:, ti, :]),
              r=[(yacc, ti)], w=[(out_key, ti)], dma=(yacc, ti))
    if after is not None:
        after(out_key)


def mm(C, out_ap, lhsT, rhs, start, stop, r, w):
    C.S.add("pe", lambda e: e.matmul(out_ap, lhsT, rhs, start=start, stop=stop), r=r, w=w)


def load_norm_T(C, src_ap, src_key, g_bc, scr, hbuf, hnb, hnT, tbank, dkey):
    S = C.S
    S.add("sp", lambda e: e.dma_start(out=hbuf[:], in_=src_ap), r=[src_key], w=[hbuf], dma=dkey)
    rmsnorm_tile(C, hbuf[:], hbuf, g_bc, [(hnb[:], hnb)], scr)
    transpose_to(C, hnb, hnT, 8, tbank)


def transpose_to(C, src, dstT, n, tbank, eng="act"):
    S = C.S
    pbv = tbank[:].bitcast(BF16)
    for kc in range(n):
        S.add("pe", lambda e, kc=kc: e.transpose(pbv[:, kc * 128:(kc + 1) * 128],
                                                  src[:, kc * 128:(kc + 1) * 128], C.identb[:]),
              r=[src, C.identb], w=[tbank])
    if eng == "act":
        S.add("act", lambda e: e.activation(dstT[:, 0:n, :], pbv[:, 0:n * 128].rearrange("p (a b) -> p a b", a=n),
                                            AF.Copy), r=[tbank], w=[dstT])
    else:
        S.add("dve", lambda e: e.tensor_copy(dstT[:, 0:n, :], pbv[:, 0:n * 128].rearrange("p (a b) -> p a b", a=n)),
              r=[tbank], w=[dstT])


def load_weight_bf16(C, dst, src, ncols, key):
    c0 = 0
    while c0 < ncols:
        n = min(1536, ncols - c0)
        C.S.add("pool", lambda e, c0=c0, n=n: e.dma_start(
            out=dst[:, :, c0:c0 + n], in_=src[:, c0:c0 + n].rearrange("(kc p) c -> p kc c", p=128)),
            w=[(dst, c0)], dma=(dst, c0))
        c0 += n


def mlstm_phase(C, h_full, h_own, out_dst, g_dram, w_in, b_i, b_f, g_out, w_out, M0d, M1d, nstep=NTO):
    S = C.S
    bank = C.bank
    W = C.sb([128, 8, 3080], BF16, "mW")
    Wo = C.sb([128, 8, D], BF16, "mWo")
    load_weight_bf16(C, W, w_in, 3080, "mW")
    load_weight_bf16(C, Wo, w_out, D, "mWo")
    g_bc = C.sb([128, D], F32, "m_gbc")
    go_bc = C.sb([128, D], F32, "m_gobc")
    bb = C.sb([128, 8], F32, "m_bb")
    M = [C.sb([128, 128], F32, "m_M%d" % i) for i in range(2)]
    S.add("sp", lambda e: e.dma_start(out=g_bc[:], in_=g_dram.partition_broadcast(128)), w=[g_bc], dma=g_bc)
    S.add("sp", lambda e: e.dma_start(out=go_bc[:], in_=g_out.partition_broadcast(128)), w=[go_bc], dma=go_bc)
    S.add("sp", lambda e: e.dma_start(out=bb[:, 0:4], in_=b_i.partition_broadcast(128)), w=[(bb, 0)], dma=(bb, 0))
    S.add("sp", lambda e: e.dma_start(out=bb[:, 4:8], in_=b_f.partition_broadcast(128)), w=[(bb, 1)], dma=(bb, 1))
    S.add("sp", lambda e: e.dma_start(out=M[0][:], in_=M0d), w=[M[0]], dma=M[0])
    S.add("sp", lambda e: e.dma_start(out=M[1][:], in_=M1d), w=[M[1]], dma=M[1])
    tri = C.sb([128, 128], F32, "m_tri")
    S.add("pool", lambda e: e.affine_select(tri[:], C.ones32[:], [[1, 128]], ALU.is_ge, 0.0, base=0,
                                            channel_multiplier=-1), r=[C.ones32], w=[tri])
    c0t = C.sb([128, 1], F32, "m_c0")
    onet = C.sb([128, 1], F32, "m_one")
    S.add("pool", lambda e: e.memset(c0t[:], float(-0.5 * np.log(128.0))), w=[c0t])
    S.add("pool", lambda e: e.memset(onet[:], 1.0), w=[onet])
    scr = norm_scratch(C)
    CT = [C.sb([128, 257], F32, "m_CT%d" % h) for h in range(4)]
    CTb = [C.sb([128, 257], BF16, "m_CTb%d" % h) for h in range(4)]
    for h in range(4):
        S.add("pool", lambda e, h=h: e.memset(CT[h][:], 0.0), w=[CT[h]])
        S.add("pool", lambda e, h=h: e.memset(CTb[h][:], 0.0), w=[CTb[h]])
    vext = [C.sb([128, 4, 257], BF16, "m_vext%d" % x) for x in range(2)]
    for x in range(2):
        S.add("pool", lambda e, x=x: e.memset(vext[x][:, :, 256:257], 1.0), w=[(vext[x], "one")])
    hk = [C.sb([128, D], F32, "m_hk%d" % x) for x in range(2)]
    hnbk = [C.sb([128, D], BF16, "m_hnbk%d" % x) for x in range(2)]
    hnTk = [C.sb([128, 8, 128], BF16, "m_hnTk%d" % x) for x in range(2)]
    kb = [C.sb([128, 512], BF16, "m_kb%d" % x) for x in range(2)]
    kT = [C.sb([128, 4, 128], BF16, "m_kT%d" % x) for x in range(2)]
    kpp = [C.sb([128, 512], BF16, "m_kpp%d" % x) for x in range(2)]
    gx = [C.sb([128, 8], F32, "m_gx%d" % x) for x in range(2)]
    tg = [C.sb([128, 8], F32, "m_tg%d" % x) for x in range(2)]
    ef = [C.sb([128, 4], F32, "m_ef%d" % x) for x in range(2)]
    spl = [C.sb([128, 4], F32, "m_sp%d" % x) for x in range(2)]
    lf = [C.sb([128, 4], F32, "m_lf%d" % x) for x in range(2)]
    dd = [C.sb([128, 4], F32, "m_dd%d" % x) for x in range(2)]
    dd2 = [C.sb([128, 4], F32, "m_dd2%d" % x) for x in range(2)]
    wexp = [C.sb([128, 4], F32, "m_wexp%d" % x) for x in range(2)]
    wst = [C.sb([128, 4], F32, "m_wst%d" % x) for x in range(2)]
    Sm = [C.sb([128, 128], BF16, "m_Sm%d" % x) for x in range(2)]
    cs = C.sb([128, 32], F32, "m_cs")
    ebl = C.sb([128, 4], F32, "m_ebl")
    ebq = C.sb([128, 4], F32, "m_ebq")
    hq = C.sb([128, D], F32, "m_hq")
    hnbq = C.sb([128, D], BF16, "m_hnbq")
    hnTq = C.sb([128, 8, 128], BF16, "m_hnTq")
    qb = C.sb([128, 512], BF16, "m_qb")
    qT = C.sb([128, 4, 128], BF16, "m_qT")
    so = C.sb([128, D], F32, "m_so")
    sog = C.sb([128, D], F32, "m_sog")
    hout = C.sb([128, D], F32, "m_hout")
    sqv = C.sb([128, D], F32, "m_sqv")
    ss4 = C.sb([128, 4], F32, "m_ss4")
    sq4 = C.sb([128, 4], F32, "m_sq4")
    r4 = C.sb([128, 4], F32, "m_r4")
    yb = C.sb([128, D], BF16, "m_yb")
    yT = C.sb([128, 8, 128], BF16, "m_yT")
    dn = C.sb([128, 1], F32, "m_dn")
    dn2 = C.sb([128, 1], F32, "m_dn2")
    rc = C.sb([128, 1], F32, "m_rc")
    scl = C.sb([128, 1], F32, "m_scl")

    for j in range(nstep):
        for x in range(2):
            ti = 2 * j + x
            load_norm_T(C, h_full(ti), ("hfull", ti), g_bc, scr, hk[x], hnbk[x],
                        hnTk[x], bank[0], hk[x])
            for kc in range(8):
                mm(C, bank[1][:, 0:512], hnTk[x][:, kc, :], W[:, kc, 512:1024], kc == 0, kc == 7,
                   [hnTk[x], W], [bank[1]])
            S.add("act", lambda e, x=x: e.activation(kb[x][:], bank[1][:, 0:512], AF.Copy), r=[bank[1]], w=[kb[x]])
            for half in range(2):
                for kc in range(8):
                    mm(C, bank[2 + half][:, 0:512], hnTk[x][:, kc, :],
                       W[:, kc, 1024 + half * 512:1024 + (half + 1) * 512], kc == 0, kc == 7,
                       [hnTk[x], W], [bank[2 + half]])
                S.add("act", lambda e, x=x, half=half: e.activation(
                    vext[x][:, 2 * half:2 * half + 2, 0:256],
                    bank[2 + half][:, 0:512].rearrange("p (a b) -> p a b", a=2), AF.Copy),
                    r=[bank[2 + half]], w=[(vext[x], half)])
            for kc in range(8):
                mm(C, bank[4][:, 0:8], hnTk[x][:, kc, :], W[:, kc, 3072:3080], kc == 0, kc == 7,
                   [hnTk[x], W], [bank[4]])
            S.add("dve", lambda e, x=x: e.tensor_tensor(gx[x][:], bank[4][:, 0:8], bb[:], ALU.add),
                  r=[bank[4], bb], w=[gx[x]])
            S.add("act", lambda e, x=x: e.activation(tg[x][:], gx[x][:], AF.Tanh, scale=1.0 / 15.0),
                  r=[gx[x]], w=[tg[x]])
            S.add("act", lambda e, x=x: e.activation(ef[x][:], tg[x][:, 4:8], AF.Exp, scale=-15.0),
                  r=[tg[x]], w=[ef[x]])
            S.add("act", lambda e, x=x: e.activation(spl[x][:], ef[x][:], AF.Ln, bias=onet[:], scale=1.0),
                  r=[ef[x], onet], w=[spl[x]])
            S.add("dve", lambda e, x=x: e.tensor_scalar(lf[x][:], spl[x][:], -1.0, None, ALU.mult),
                  r=[spl[x]], w=[lf[x]])
            transpose_to(C, kb[x], kT[x], 4, bank[0], eng="dve")
        b5 = bank[5]
        mm(C, b5[:, 0:4], tri[:], lf[0][:], True, True, [tri, lf[0]], [b5])
        mm(C, b5[:, 8:12], C.ones32[:], lf[0][:], True, False, [C.ones32, lf[0]], [b5])
        mm(C, b5[:, 8:12], tri[:], lf[1][:], False, True, [tri, lf[1]], [b5])
        mm(C, b5[:, 16:20], C.ones32[:], lf[0][:], True, False, [C.ones32, lf[0]], [b5])
        mm(C, b5[:, 16:20], C.ones32[:], lf[1][:], False, True, [C.ones32, lf[1]], [b5])
        mm(C, b5[:, 24:28], M[0][:], lf[0][:], True, False, [M[0], lf[0]], [b5])
        mm(C, b5[:, 24:28], M[1][:], lf[1][:], False, True, [M[1], lf[1]], [b5])
        S.add("dve", lambda e: e.tensor_copy(cs[:], b5[:, 0:32]), r=[b5], w=[cs])
        for x in range(2):
            S.add("dve", lambda e, x=x: e.scalar_tensor_tensor(dd[x][:], tg[x][:, 0:4], 15.0, cs[:, 8 * x:8 * x + 4],
                                                               ALU.mult, ALU.subtract),
                  r=[tg[x], cs], w=[dd[x]])
            S.add("act", lambda e, x=x: e.activation(wexp[x][:], dd[x][:], AF.Exp, bias=c0t[:], scale=1.0),
                  r=[dd[x], c0t], w=[wexp[x]])
            S.add("dve", lambda e, x=x: e.tensor_tensor(dd2[x][:], dd[x][:], cs[:, 16:20], ALU.add),
                  r=[dd[x], cs], w=[dd2[x]])
            S.add("act", lambda e, x=x: e.activation(wst[x][:], dd2[x][:], AF.Exp, bias=c0t[:], scale=1.0),
                  r=[dd2[x], c0t], w=[wst[x]])
            for h in range(4):
                S.add("dve", lambda e, x=x, h=h: e.tensor_scalar(
                    kpp[x][:, h * 128:(h + 1) * 128], kb[x][:, h * 128:(h + 1) * 128], wst[x][:, h:h + 1], None,
                    ALU.mult), r=[kb[x], wst[x]], w=[(kpp[x], h)])
        S.add("act", lambda e: e.activation(ebl[:], cs[:, 16:20], AF.Exp), r=[cs], w=[ebl])
        S.add("act", lambda e: e.activation(ebq[:], cs[:, 24:28], AF.Exp), r=[cs], w=[ebq])
        load_norm_T(C, h_own[j * 128:(j + 1) * 128, :], ("hown", j), g_bc, scr, hq, hnbq, hnTq, bank[0], hq)
        for kc in range(8):
            mm(C, bank[1][:, 0:512], hnTq[:, kc, :], W[:, kc, 0:512], kc == 0, kc == 7, [hnTq, W], [bank[1]])
        S.add("act", lambda e: e.activation(qb[:], bank[1][:, 0:512], AF.Copy), r=[bank[1]], w=[qb])
        transpose_to(C, qb, qT, 4, bank[0], eng="dve")
        for half in range(2):
            for kc in range(8):
                mm(C, bank[2 + half][:, 0:512], hnTq[:, kc, :], W[:, kc, 2048 + half * 512:2048 + (half + 1) * 512],
                   kc == 0, kc == 7, [hnTq, W], [bank[2 + half]])
            S.add("act", lambda e, half=half: e.activation(so[:, half * 512:(half + 1) * 512],
                                                           bank[2 + half][:, 0:512], AF.Sigmoid),
                  r=[bank[2 + half]], w=[(so, half)])
        S.add("dve", lambda e: e.tensor_tensor(sog[:], so[:], go_bc[:], ALU.mult), r=[so, go_bc], w=[sog])
        for h in range(4):
            for x in range(2):
                mm(C, bank[6][:, x * 128:(x + 1) * 128], kT[x][:, h, :], qT[:, h, :], True, True,
                   [kT[x], qT], [(bank[6], x)])
                S.add("dve", lambda e, x=x, h=h: e.scalar_tensor_tensor(
                    Sm[x][:], bank[6][:, x * 128:(x + 1) * 128], wexp[x][:, h:h + 1], M[x][:], ALU.mult, ALU.mult),
                    r=[(bank[6], x), wexp[x], M[x]], w=[Sm[x]])
            nb = bank[2 + (h % 2)]
            mm(C, nb[:, 0:257], Sm[0][:], vext[0][:, h, :], True, False, [Sm[0], vext[0]], [nb])
            mm(C, nb[:, 0:257], Sm[1][:], vext[1][:, h, :], False, False, [Sm[1], vext[1]], [nb])
            mm(C, nb[:, 0:257], qT[:, h, :], CTb[h][:], False, True, [qT, CTb[h]], [nb])
            S.add("dve", lambda e, nb=nb, h=h: e.tensor_tensor(dn[:], nb[:, 256:257], ebq[:, h:h + 1], ALU.mult),
                  r=[nb, ebq], w=[dn])
            S.add("dve", lambda e: e.tensor_scalar(dn2[:], dn[:], -1.0, 1.0, ALU.mult, ALU.max), r=[dn], w=[dn2])
            S.add("dve", lambda e: e.tensor_tensor(dn2[:], dn2[:], dn[:], ALU.max), r=[dn, dn2], w=[dn2])
            S.add("dve", lambda e: e.reciprocal(rc[:], dn2[:]), r=[dn2], w=[rc])
            S.add("dve", lambda e, h=h: e.tensor_tensor(scl[:], rc[:], ebq[:, h:h + 1], ALU.mult),
                  r=[rc, ebq], w=[scl])
            S.add("dve", lambda e, nb=nb, h=h: e.tensor_scalar(hout[:, h * 256:(h + 1) * 256], nb[:, 0:256],
                                                               scl[:, 0:1], None, ALU.mult),
                  r=[nb, scl], w=[(hout, h)])
        S.add("act", lambda e: e.activation(sqv[:], hout[:], AF.Square), r=[hout], w=[sqv])
        S.add("dve", lambda e: e.tensor_reduce(ss4[:], sqv[:].rearrange("p (a b) -> p a b", a=4), AX.X, ALU.add),
              r=[sqv], w=[ss4])
        S.add("act", lambda e: e.activation(sq4[:], ss4[:], AF.Sqrt, bias=scr["eps"][:], scale=1.0 / 256.0),
              r=[ss4, scr["eps"]], w=[sq4])
        S.add("dve", lambda e: e.reciprocal(r4[:], sq4[:]), r=[sq4], w=[r4])
        for h in range(4):
            S.add("dve", lambda e, h=h: e.scalar_tensor_tensor(
                yb[:, h * 256:(h + 1) * 256], hout[:, h * 256:(h + 1) * 256], r4[:, h:h + 1],
                sog[:, h * 256:(h + 1) * 256], ALU.mult, ALU.mult), r=[hout, r4, sog], w=[(yb, h)])
        transpose_to(C, yb, yT, 8, bank[0])
        for half in range(2):
            for kc in range(8):
                mm(C, bank[2 + half][:, 0:512], yT[:, kc, :], Wo[:, kc, half * 512:(half + 1) * 512],
                   kc == 0, kc == 7, [yT, Wo], [bank[2 + half]])
            S.add("dve", lambda e, half=half: e.tensor_tensor(hq[:, half * 512:(half + 1) * 512],
                                                              bank[2 + half][:, 0:512],
                                                              hq[:, half * 512:(half + 1) * 512], ALU.add),
                  r=[bank[2 + half], hq], w=[hq])
        S.add("sp", lambda e, j=j: e.dma_start(out=out_dst[j * 128:(j + 1) * 128, :], in_=hq[:]),
              r=[hq], w=[("mout", j)], dma=hq)
        for h in range(4):
            mm(C, bank[7][:, 0:257], kpp[0][:, h * 128:(h + 1) * 128], vext[0][:, h, :], True, False,
               [kpp[0], vext[0]], [bank[7]])
            mm(C, bank[7][:, 0:257], kpp[1][:, h * 128:(h + 1) * 128], vext[1][:, h, :], False, True,
               [kpp[1], vext[1]], [bank[7]])
            S.add("dve", lambda e, h=h: e.scalar_tensor_tensor(CT[h][:], CT[h][:], ebl[:, h:h + 1],
                                                               bank[7][:, 0:257], ALU.mult, ALU.add),
                  r=[CT[h], ebl, bank[7]], w=[CT[h]])
            S.add("act", lambda e, h=h: e.activation(CTb[h][:], CT[h][:], AF.Copy), r=[CT[h]], w=[CTb[h]])


def rope_ops(C, x1, x2, cos, sin, o1, o2, ta, tb, rres, wres, wkey=0):
    S = C.S
    S.add("dve", lambda e: e.tensor_tensor(ta, x1, cos, ALU.mult), r=rres, w=[("ropeta", 0)])
    S.add("dve", lambda e: e.tensor_tensor(tb, x2, sin, ALU.mult), r=rres, w=[("ropetb", 0)])
    S.add("dve", lambda e: e.tensor_tensor(o1, ta, tb, ALU.subtract), r=[("ropeta", 0), ("ropetb", 0)],
          w=[(wres, (wkey, "a"))])
    S.add("dve", lambda e: e.tensor_tensor(ta, x2, cos, ALU.mult), r=rres, w=[("ropeta", 0)])
    S.add("dve", lambda e: e.tensor_tensor(tb, x1, sin, ALU.mult), r=rres, w=[("ropetb", 0)])
    S.add("dve", lambda e: e.tensor_tensor(o2, ta, tb, ALU.add), r=[("ropeta", 0), ("ropetb", 0)],
          w=[(wres, (wkey, "b"))])


def dsa_phase(C, h_full, h_own, out_dst, g_dram, w_in, gq_d, gk_d, w_out, tabk, tabq, M0d, M1d, pen_d,
              nstep=NTO, nbis=24):
    S = C.S
    bank = C.bank
    NTK = 2 * nstep
    LPK = NTK * 128
    W = C.sb([128, 8, 2696], BF16, "aW")
    Wo = C.sb([128, 8, D], BF16, "aWo")
    load_weight_bf16(C, W, w_in, 2696, "aW")
    load_weight_bf16(C, Wo, w_out, D, "aWo")
    g_bc = C.sb([128, D], F32, "a_gbc")
    gq_bc = C.sb([128, 64], F32, "a_gq")
    gk_bc = C.sb([128, 64], F32, "a_gk")
    pen = C.sb([128, 256], F32, "a_pen")
    S.add("sp", lambda e: e.dma_start(out=g_bc[:], in_=g_dram.partition_broadcast(128)), w=[g_bc], dma=g_bc)
    S.add("sp", lambda e: e.dma_start(out=gq_bc[:], in_=gq_d.partition_broadcast(128)), w=[gq_bc], dma=gq_bc)
    S.add("sp", lambda e: e.dma_start(out=gk_bc[:], in_=gk_d.partition_broadcast(128)), w=[gk_bc], dma=gk_bc)
    S.add("sp", lambda e: e.dma_start(out=pen[:], in_=pen_d), w=[pen], dma=pen)
    scr = norm_scratch(C)
    mq = C.sb([128, 1], F32, "a_mq")
    mk_ = C.sb([128, 1], F32, "a_mk")
    negM = C.sb([128, 1], F32, "a_negM")
    S.add("dve", lambda e: e.tensor_reduce(mq[:], gq_bc[:], AX.X, ALU.max, apply_absolute_value=True),
          r=[gq_bc], w=[mq])
    S.add("dve", lambda e: e.tensor_reduce(mk_[:], gk_bc[:], AX.X, ALU.max, apply_absolute_value=True),
          r=[gk_bc], w=[mk_])
    S.add("dve", lambda e: e.tensor_tensor(negM[:], mq[:], mk_[:], ALU.mult), r=[mq, mk_], w=[negM])
    S.add("dve", lambda e: e.tensor_scalar(negM[:], negM[:], -8.0, None, ALU.mult), r=[negM], w=[negM])
    kT_all = C.sb([64, 4, LPK], BF16, "a_kT")
    vext = C.sb([128, NTK, 4, 65], BF16, "a_vext")
    ikT = C.sb([128, LPK], BF16, "a_ikT")
    S.add("pool", lambda e: e.memset(vext[:, :, :, 64:65], 1.0), w=[(vext, "one")])
    score = C.sb([128, LPK], F32, "a_score")
    junk = C.sb([128, LPK], BF16, "a_junk")
    ck64 = C.sb([128, 2, 32], F32, "a_ck64")
    sk64 = C.sb([128, 2, 32], F32, "a_sk64")
    ck128 = C.sb([128, 2, 64], F32, "a_ck128")
    sk128 = C.sb([128, 2, 64], F32, "a_sk128")
    cq64 = C.sb([128, 32], F32, "a_cq64")
    sq64 = C.sb([128, 32], F32, "a_sq64")
    cq128 = C.sb([128, 64], F32, "a_cq128")
    sq128 = C.sb([128, 64], F32, "a_sq128")
    hk = C.sb([128, D], F32, "a_hk")
    hnbk = C.sb([128, D], BF16, "a_hnbk")
    hnTk = C.sb([128, 8, 128], BF16, "a_hnTk")
    sqk = C.sb([128, 256], F32, "a_sqk")
    ssk = C.sb([128, 4], F32, "a_ssk")
    srk = C.sb([128, 4], F32, "a_srk")
    rk = C.sb([128, 4], F32, "a_rk")
    kn = C.sb([128, 256], F32, "a_kn")
    krot = C.sb([128, 256], BF16, "a_krot")
    ikrot = C.sb([128, 128], BF16, "a_ikrot")
    ta = C.sb([128, 512], F32, "a_ta")
    tb = C.sb([128, 512], F32, "a_tb")
    hq = C.sb([128, D], F32, "a_hq")
    hnbq = C.sb([128, D], BF16, "a_hnbq")
    hnTq = C.sb([128, 8, 128], BF16, "a_hnTq")
    ssq = C.sb([128, 16], F32, "a_ssq")
    srq = C.sb([128, 16], F32, "a_srq")
    rq = C.sb([128, 16], F32, "a_rq")
    qn = C.sb([128, D], F32, "a_qn")
    sqq = qn
    qrot = C.sb([128, D], BF16, "a_qrot")
    iqrot = hnbq
    qT = C.sb([64, 16, 128], BF16, "a_qT")
    iqT = C.sb([128, 8, 128], BF16, "a_iqT")
    iw = C.sb([128, 8], F32, "a_iw")
    rl = [C.sb([128, 512], F32, "a_rl%d" % i) for i in range(2)]
    amax = C.sb([128, 1], F32, "a_amax")
    w0 = C.sb([128, 1], F32, "a_w0")
    lo = C.sb([128, 1], F32, "a_lo")
    mid = C.sb([128, 1], F32, "a_mid")
    halfs = C.sb([128, nbis], F32, "a_halfs")
    cnt = C.sb([128, 1], F32, "a_cnt")
    stp = C.sb([128, 1], F32, "a_stp")
    dg = C.sb([128, 128], F32, "a_dg")
    thrbc = C.sb([128, 128], F32, "a_thrbc")
    mkt = [C.sb([128, 128], BF16, "a_mk%d" % i) for i in range(2)]
    pt = [C.sb([128, 4, 128], BF16, "a_pt%d" % i) for i in range(2)]
    ptm = [C.sb([128, 4, 128], BF16, "a_ptm%d" % i) for i in range(2)]
    rd = C.sb([128, 16], F32, "a_rd")
    accs = C.sb([128, 16 * 65], F32, "a_accs")
    attn = qrot
    attnT = hnTq
    pbv0 = bank[0][:].bitcast(BF16)
    tk = [t for t in tabk]
    tq = [t for t in tabq]

    def bc(ap, h):
        return ap.unsqueeze(1).broadcast_to([128, h, ap.shape[-1]])

    for j in range(nstep):
        r0 = 2 * j * 128
        for dst, src in ((ck64, tk[0]), (sk64, tk[1]), (ck128, tk[2]), (sk128, tk[3])):
            S.add("sp", lambda e, dst=dst, src=src, r0=r0: e.dma_start(
                out=dst[:], in_=src[r0:r0 + 256, :].rearrange("(x p) d -> p x d", p=128)), w=[dst], dma=dst)
        for x in range(2):
            ti = 2 * j + x
            load_norm_T(C, h_full(ti), ("hfull", ti), g_bc, scr, hk, hnbk, hnTk,
                        bank[0], hk)
            for kc in range(8):
                mm(C, bank[1][:, 0:512], hnTk[:, kc, :], W[:, kc, 1024:1536], kc == 0, kc == 7, [hnTk, W], [bank[1]])
            for kc in range(8):
                mm(C, bank[4][:, 0:128], hnTk[:, kc, :], W[:, kc, 2560:2688], kc == 0, kc == 7, [hnTk, W], [bank[4]])
            S.add("act", lambda e, ti=ti: e.activation(
                vext[:, ti, :, 0:64], bank[1][:, 256:512].rearrange("p (a b) -> p a b", a=4), AF.Copy),
                r=[bank[1]], w=[(vext, ti)])
            S.add("act", lambda e: e.activation(sqk[:], bank[1][:, 0:256], AF.Square), r=[bank[1]], w=[sqk])
            S.add("dve", lambda e: e.tensor_reduce(ssk[:], sqk[:].rearrange("p (a b) -> p a b", a=4), AX.X, ALU.add),
                  r=[sqk], w=[ssk])
            S.add("act", lambda e: e.activation(srk[:], ssk[:], AF.Sqrt, bias=scr["eps"][:], scale=1.0 / 64.0),
                  r=[ssk, scr["eps"]], w=[srk])
            S.add("dve", lambda e: e.reciprocal(rk[:], srk[:]), r=[srk], w=[rk])
            kn3 = kn[:].rearrange("p (a b) -> p a b", a=4)
            S.add("dve", lambda e, kn3=kn3: e.tensor_tensor(
                kn3, bank[1][:, 0:256].rearrange("p (a b) -> p a b", a=4),
                rk[:].unsqueeze(2).broadcast_to([128, 4, 64]), ALU.mult), r=[bank[1], rk], w=[kn])
            S.add("dve", lambda e, kn3=kn3: e.tensor_tensor(kn3, kn3, bc(gk_bc[:], 4), ALU.mult),
                  r=[kn, gk_bc], w=[kn])
            kr3 = krot[:].rearrange("p (a b) -> p a b", a=4)
            ta3 = ta[:, 0:128].rearrange("p (a b) -> p a b", a=4)
            tb3 = tb[:, 0:128].rearrange("p (a b) -> p a b", a=4)
            rope_ops(C, kn3[:, :, 0:32], kn3[:, :, 32:64], bc(ck64[:, x, :], 4), bc(sk64[:, x, :], 4),
                     kr3[:, :, 0:32], kr3[:, :, 32:64], ta3, tb3, [kn, ck64, sk64], krot)
            for h in range(4):
                S.add("pe", lambda e, h=h: e.transpose(pbv0[0:64, h * 128:(h + 1) * 128],
                                                       krot[:, h * 64:(h + 1) * 64], C.identb[:]),
                      r=[krot, C.identb], w=[bank[0]])
            S.add("act", lambda e, ti=ti: e.activation(
                kT_all[:, :, ti * 128:(ti + 1) * 128], pbv0[0:64, 0:512].rearrange("p (a b) -> p a b", a=4),
                AF.Copy), r=[bank[0]], w=[(kT_all, ti)])
            rope_ops(C, bank[4][:, 0:64], bank[4][:, 64:128], ck128[:, x, :], sk128[:, x, :],
                     ikrot[:, 0:64], ikrot[:, 64:128], ta[:, 0:64], tb[:, 0:64], [bank[4], ck128, sk128], ikrot)
            S.add("pe", lambda e: e.transpose(pbv0[:, 0:128], ikrot[:], C.identb[:]),
                  r=[ikrot, C.identb], w=[bank[0]])
            S.add("act", lambda e, ti=ti: e.activation(ikT[:, ti * 128:(ti + 1) * 128], pbv0[:, 0:128], AF.Copy),
                  r=[bank[0]], w=[(ikT, ti)])
        q0 = j * 128
        for dst, src in ((cq64, tq[0]), (sq64, tq[1]), (cq128, tq[2]), (sq128, tq[3])):
            S.add("sp", lambda e, dst=dst, src=src, q0=q0: e.dma_start(out=dst[:], in_=src[q0:q0 + 128, :]),
                  w=[dst], dma=dst)
        load_norm_T(C, h_own[q0:q0 + 128, :], ("hown", j), g_bc, scr, hq, hnbq, hnTq, bank[0], hq)
        for half in range(2):
            for kc in range(8):
                mm(C, bank[2 + half][:, 0:512], hnTq[:, kc, :], W[:, kc, half * 512:(half + 1) * 512],
                   kc == 0, kc == 7, [hnTq, W], [bank[2 + half]])
            S.add("act", lambda e, half=half: e.activation(sqq[:, half * 512:(half + 1) * 512],
                                                           bank[2 + half][:, 0:512], AF.Square),
                  r=[bank[2 + half]], w=[(sqq, half)])
        S.add("dve", lambda e: e.tensor_reduce(ssq[:], sqq[:].rearrange("p (a b) -> p a b", a=16), AX.X, ALU.add),
              r=[sqq], w=[ssq])
        S.add("act", lambda e: e.activation(srq[:], ssq[:], AF.Sqrt, bias=scr["eps"][:], scale=1.0 / 64.0),
              r=[ssq, scr["eps"]], w=[srq])
        S.add("dve", lambda e: e.reciprocal(rq[:], srq[:]), r=[srq], w=[rq])
        qn3 = qn[:].rearrange("p (a b) -> p a b", a=16)
        for half in range(2):
            S.add("dve", lambda e, half=half: e.tensor_tensor(
                qn3[:, half * 8:(half + 1) * 8, :], bank[2 + half][:, 0:512].rearrange("p (a b) -> p a b", a=8),
                rq[:, half * 8:(half + 1) * 8].unsqueeze(2).broadcast_to([128, 8, 64]), ALU.mult),
                r=[bank[2 + half], rq], w=[(qn, half)])
        S.add("dve", lambda e: e.tensor_tensor(qn3, qn3, bc(gq_bc[:], 16), ALU.mult), r=[qn, gq_bc], w=[qn])
        qr3 = qrot[:].rearrange("p (a b) -> p a b", a=16)
        ta16 = ta[:, 0:512].rearrange("p (a b) -> p a b", a=16)
        tb16 = tb[:, 0:512].rearrange("p (a b) -> p a b", a=16)
        rope_ops(C, qn3[:, :, 0:32], qn3[:, :, 32:64], bc(cq64[:], 16), bc(sq64[:], 16),
                 qr3[:, :, 0:32], qr3[:, :, 32:64], ta16, tb16, [qn, cq64, sq64], qrot)
        for hb in range(2):
            pbv = bank[hb][:].bitcast(BF16)
            for h in range(8):
                hh = hb * 8 + h
                S.add("pe", lambda e, pbv=pbv, h=h, hh=hh: e.transpose(
                    pbv[0:64, h * 128:(h + 1) * 128], qrot[:, hh * 64:(hh + 1) * 64], C.identb[:]),
                    r=[qrot, C.identb], w=[bank[hb]])
            S.add("act", lambda e, pbv=pbv, hb=hb: e.activation(
                qT[:, hb * 8:(hb + 1) * 8, :], pbv[0:64, 0:1024].rearrange("p (a b) -> p a b", a=8), AF.Copy),
                r=[bank[hb]], w=[(qT, hb)])
        for half in range(2):
            for kc in range(8):
                mm(C, bank[2 + half][:, 0:512], hnTq[:, kc, :], W[:, kc, 1536 + half * 512:1536 + (half + 1) * 512],
                   kc == 0, kc == 7, [hnTq, W], [bank[2 + half]])
        for kc in range(8):
            mm(C, bank[4][:, 0:8], hnTq[:, kc, :], W[:, kc, 2688:2696], kc == 0, kc == 7, [hnTq, W], [bank[4]])
        S.add("dve", lambda e: e.tensor_scalar(iw[:], bank[4][:, 0:8], 1.0 / 32.0, None, ALU.mult),
              r=[bank[4]], w=[iw])
        iq3r = iqrot[:].rearrange("p (a b) -> p a b", a=8)
        for half in range(2):
            src3 = bank[2 + half][:, 0:512].rearrange("p (a b) -> p a b", a=4)
            o3 = iq3r[:, half * 4:(half + 1) * 4, :]
            ta4 = ta[:, 0:256].rearrange("p (a b) -> p a b", a=4)
            tb4 = tb[:, 0:256].rearrange("p (a b) -> p a b", a=4)
            rope_ops(C, src3[:, :, 0:64], src3[:, :, 64:128], bc(cq128[:], 4), bc(sq128[:], 4),
                     o3[:, :, 0:64], o3[:, :, 64:128], ta4, tb4, [bank[2 + half], cq128, sq128], iqrot, wkey=half)
        transpose_to(C, iqrot, iqT, 8, bank[0])
        NK = (2 * j + 2) * 128
        k0 = 0
        blk = 0
        while k0 < NK:
            kn_ = min(512, NK - k0)
            for h in range(8):
                pb = bank[4 + blk % 2]
                rb = rl[blk % 2]
                blk += 1
                mm(C, pb[:, 0:kn_], iqT[:, h, :], ikT[:, k0:k0 + kn_], True, True, [iqT, ikT], [pb])
                S.add("act", lambda e, pb=pb, rb=rb, kn_=kn_: e.activation(rb[:, 0:kn_], pb[:, 0:kn_], AF.Relu),
                      r=[pb], w=[rb])
                if h == 0:
                    S.add("dve", lambda e, rb=rb, k0=k0, kn_=kn_: e.tensor_scalar(
                        score[:, k0:k0 + kn_], rb[:, 0:kn_], iw[:, 0:1], None, ALU.mult),
                        r=[rb, iw], w=[(score, k0)])
                else:
                    S.add("dve", lambda e, rb=rb, k0=k0, kn_=kn_, h=h: e.scalar_tensor_tensor(
                        score[:, k0:k0 + kn_], rb[:, 0:kn_], iw[:, h:h + 1], score[:, k0:k0 + kn_],
                        ALU.mult, ALU.add), r=[rb, iw, (score, k0)], w=[(score, k0)])
            k0 += kn_
        S.add("dve", lambda e, NK=NK: e.tensor_reduce(amax[:], score[:, 0:NK], AX.X, ALU.max,
                                                      apply_absolute_value=True), r=[score], w=[amax])
        S.add("dve", lambda e, NK=NK: e.tensor_tensor(score[:, NK - 256:NK], score[:, NK - 256:NK], pen[:], ALU.add),
              r=[score, pen], w=[score])
        S.add("dve", lambda e: e.tensor_scalar(lo[:], amax[:], -1.0, -1.0, ALU.mult, ALU.add), r=[amax], w=[lo])
        S.add("dve", lambda e: e.tensor_scalar(w0[:], amax[:], 2.0, 2.0, ALU.mult, ALU.add), r=[amax], w=[w0])
        for it in range(nbis):
            S.add("dve", lambda e, it=it: e.tensor_scalar(halfs[:, it:it + 1], w0[:], float(2.0 ** -(it + 1)), None,
                                                          ALU.mult), r=[w0], w=[(halfs, it)])
        for it in range(nbis):
            S.add("dve", lambda e, it=it: e.tensor_tensor(mid[:], lo[:], halfs[:, it:it + 1], ALU.add),
                  r=[lo, (halfs, it)], w=[mid])
            S.add("dve", lambda e, NK=NK: e.tensor_scalar(junk[:, 0:NK], score[:, 0:NK], mid[:, 0:1], 0.0,
                                                          ALU.is_ge, ALU.add, accum_out=cnt[:]),
                  r=[score, mid], w=[junk, cnt])
            S.add("dve", lambda e, it=it: e.scalar_tensor_tensor(stp[:], cnt[:], 256.0, halfs[:, it:it + 1],
                                                                 ALU.is_ge, ALU.mult),
                  r=[cnt, (halfs, it)], w=[stp])
            S.add("dve", lambda e: e.tensor_tensor(lo[:], lo[:], stp[:], ALU.add), r=[lo, stp], w=[lo])
        S.add("dve", lambda e: e.tensor_scalar(dg[:], C.ident32[:], lo[:, 0:1], None, ALU.mult),
              r=[C.ident32, lo], w=[dg])
        mm(C, bank[0][:, 0:128], C.ones32[:], dg[:], True, True, [C.ones32, dg], [bank[0]])
        S.add("dve", lambda e: e.tensor_copy(thrbc[:], bank[0][:, 0:128]), r=[bank[0]], w=[thrbc])
        if getattr(C, "dbg", None) and j == C.dbg["j"]:
            dd_ = C.dbg
            S.add("sp", lambda e: e.dma_start(out=dd_["score"], in_=score[:, 0:dd_["score"].shape[1]]), r=[score],
                  w=["dbg_score"], dma=score)
            S.add("sp", lambda e: e.dma_start(out=dd_["lo"], in_=lo[:]), r=[lo], w=["dbg_lo"], dma=lo)
            S.add("sp", lambda e: e.dma_start(out=dd_["thrbc"], in_=thrbc[:]), r=[thrbc], w=["dbg_thr"], dma=thrbc)
            S.add("sp", lambda e: e.dma_start(out=dd_["amax"], in_=amax[:]), r=[amax], w=["dbg_amax"], dma=amax)
            S.add("sp", lambda e: e.dma_start(out=dd_["cnt"], in_=cnt[:]), r=[cnt], w=["dbg_cnt"], dma=cnt)
        nkt = 2 * j + 2
        accb = [bank[1], bank[2], bank[3]]
        ub = 0
        for kt in range(nkt):
            mkb = mkt[kt % 2]
            S.add("pe", lambda e, kt=kt: e.transpose(bank[0][:, 128:256], score[:, kt * 128:(kt + 1) * 128],
                                                     C.ident32[:]), r=[score, C.ident32], w=[bank[0]])
            S.add("dve", lambda e, mkb=mkb: e.tensor_tensor(mkb[:], bank[0][:, 128:256], thrbc[:], ALU.is_ge),
                  r=[bank[0], thrbc], w=[mkb])
            for g in range(4):
                sb_ = bank[6 + ub % 2]
                ptb, ptmb = pt[ub % 2], ptm[ub % 2]
                ub += 1
                mm(C, sb_[:, 0:512], kT_all[:, g, kt * 128:(kt + 1) * 128],
                   qT[:, 4 * g:4 * g + 4, :].rearrange("p a b -> p (a b)"), True, True, [kT_all, qT], [sb_])
                S.add("act", lambda e, sb_=sb_, ptb=ptb: e.activation(
                    ptb[:].rearrange("p a b -> p (a b)"), sb_[:, 0:512], AF.Exp, bias=negM[:], scale=0.125),
                    r=[sb_, negM], w=[ptb])
                S.add("dve", lambda e, ptb=ptb, ptmb=ptmb, mkb=mkb: e.tensor_tensor(
                    ptmb[:], ptb[:], bc(mkb[:], 4), ALU.mult), r=[ptb, mkb], w=[ptmb])
                for hh in range(4):
                    hd = 4 * g + hh
                    ab = accb[hd // 7]
                    c0 = (hd % 7) * 65
                    mm(C, ab[:, c0:c0 + 65], ptmb[:, hh, :], vext[:, kt, g, :], True, True,
                       [ptmb, vext], [(ab, hd)])
            for bi, (h0, nh) in enumerate(((0, 7), (7, 7), (14, 2))):
                if kt == 0:
                    S.add("dve", lambda e, bi=bi, h0=h0, nh=nh: e.tensor_copy(
                        accs[:, h0 * 65:(h0 + nh) * 65], accb[bi][:, 0:nh * 65]), r=[accb[bi]], w=[(accs, bi)])
                else:
                    S.add("dve", lambda e, bi=bi, h0=h0, nh=nh: e.tensor_tensor(
                        accs[:, h0 * 65:(h0 + nh) * 65], accb[bi][:, 0:nh * 65], accs[:, h0 * 65:(h0 + nh) * 65],
                        ALU.add), r=[accb[bi], (accs, bi)], w=[(accs, bi)])
        at3 = attn[:].rearrange("p (a b) -> p a b", a=16)
        for bi, (h0, nh) in enumerate(((0, 7), (7, 7), (14, 2))):
            a3 = accs[:, h0 * 65:(h0 + nh) * 65].rearrange("p (a b) -> p a b", a=nh)
            S.add("dve", lambda e, a3=a3, h0=h0, nh=nh: e.reciprocal(rd[:, h0:h0 + nh], a3[:, :, 64]),
                  r=[(accs, bi)], w=[(rd, bi)])
            S.add("dve", lambda e, a3=a3, h0=h0, nh=nh: e.tensor_tensor(
                at3[:, h0:h0 + nh, :], a3[:, :, 0:64], rd[:, h0:h0 + nh].unsqueeze(2).broadcast_to([128, nh, 64]),
                ALU.mult), r=[(accs, bi), (rd, bi)], w=[(attn, bi)])
        transpose_to(C, attn, attnT, 8, bank[0])
        for half in range(2):
            for kc in range(8):
                mm(C, bank[4 + half][:, 0:512], attnT[:, kc, :], Wo[:, kc, half * 512:(half + 1) * 512],
                   kc == 0, kc == 7, [attnT, Wo], [bank[4 + half]])
            S.add("dve", lambda e, half=half: e.tensor_tensor(hq[:, half * 512:(half + 1) * 512],
                                                              bank[4 + half][:, 0:512],
                                                              hq[:, half * 512:(half + 1) * 512], ALU.add),
                  r=[bank[4 + half], hq], w=[hq])
        S.add("sp", lambda e, j=j: e.dma_start(out=out_dst[j * 128:(j + 1) * 128, :], in_=hq[:]),
              r=[hq], w=[("aout", j)], dma=hq)


NCORES = 8
_PROG = None
_PLAN = None
GROUPS = [[0, 1], [2, 3], [4, 5], [6, 7]]


def _rope_tabs(pos, half):
    inv = (10000.0 ** (-np.arange(half, dtype=np.float32) / np.float32(half))).astype(np.float32)
    ang = (pos.astype(np.float32)[:, None] * inv[None, :]).astype(np.float32)
    return np.cos(ang).astype(np.float32), np.sin(ang).astype(np.float32)


def _dt(nc, n, s, k="ExternalInput"):
    return nc.dram_tensor(n, list(s), F32, kind=k).ap()


def _build_fused(plan=None):
    if plan is None:
        plan = [("dsa", 0), ("ffn", 0), ("mlstm", 1), ("moe", 1), ("dsa", 2), ("ffn", 2), ("mlstm", 3), ("moe", 3)]
    nc = bass.Bass("TRN2", target_bir_lowering=False)
    NR = NTO * 128
    decl = {}

    def inp(name, shape):
        if name not in decl:
            decl[name] = _dt(nc, name, shape)
        return decl[name]

    x0 = inp("x0", [NR, D])
    out = _dt(nc, "out", [NR, D], "ExternalOutput")
    xo_t = nc.dram_tensor("xo", [NR, D], F32)
    xa_t = nc.dram_tensor("xa", [2 * NR, D], F32)
    xo, xa = xo_t.ap(), xa_t.ap()
    xm = xo

    def hf_tile(ti):
        return xa[ti * 128:(ti + 1) * 128, :]

    def gather(C):
        def after(out_key):
            for jj in range(NTO):
                C.S.add("pool", lambda e, jj=jj: e.collective_compute(
                    "AllGather", ALU.bypass, replica_groups=GROUPS,
                    ins=[xo_t.ap()[jj * 128:(jj + 1) * 128, :].opt()],
                    outs=[xa_t.ap()[2 * jj * 128:2 * (jj + 1) * 128, :].opt()]),
                    r=[(out_key, jj)], w=[("xa", jj)], dma="cc", cc=True)
        return after

    def phase(fn):
        with nc.cleanup_on_exit():
            with ExitStack() as es:
                C = Ctx(nc, es)
                C.consts()
                fn(C)
                C.S.emit()

    def p0(C):
        for jj in range(NTO):
            C.S.add("sp", lambda e, jj=jj: e.dma_start(out=xo[jj * 128:(jj + 1) * 128, :],
                                                       in_=x0[jj * 128:(jj + 1) * 128, :]),
                    w=[("xo", jj)], dma=("cp0", jj))
        gather(C)("xo")
    phase(p0)
    for pi, (kind, i) in enumerate(plan):
        j = i // 2
        last = pi == len(plan) - 1
        if kind in ("dsa", "mlstm"):
            M0 = inp("M0", [128, 128]); M1 = inp("M1", [128, 128])
            nm = inp("norm_mixer", [4, D])
            dst = out if last else xm
        if kind in ("ffn", "moe"):
            nf = inp("norm_ffn", [4, D]); rowmask = inp("rowmask", [128, 1])
            dst = out if last else xo
            src = xm if (pi > 0 and plan[pi - 1][0] in ("dsa", "mlstm")) else xo
        if kind == "dsa":
            a_win = inp("dsa_w_in", [2, D, 2696]); a_gq = inp("dsa_q_norm", [2, 64])
            a_gk = inp("dsa_k_norm", [2, 64]); a_wo = inp("dsa_w_out", [2, D, D]); pen = inp("pen", [128, 256])
            tabk = [inp("tk%d" % t, [LP, 32 if t < 2 else 64]) for t in range(4)]
            tabq = [inp("tq%d" % t, [NR, 32 if t < 2 else 64]) for t in range(4)]
            phase(lambda C: dsa_phase(C, hf_tile, xo, dst, nm[i], a_win[j], a_gq[j], a_gk[j], a_wo[j],
                                      tabk, tabq, M0, M1, pen))
        elif kind == "mlstm":
            m_win = inp("mlstm_w_in", [2, D, 3080]); m_bi = inp("mlstm_b_i", [2, 4])
            m_bf = inp("mlstm_b_f", [2, 4]); m_go = inp("mlstm_out_norm", [2, D]); m_wo = inp("mlstm_w_out", [2, D, D])
            phase(lambda C: mlstm_phase(C, hf_tile, xo, dst, nm[i], m_win[j], m_bi[j], m_bf[j], m_go[j],
                                        m_wo[j], M0, M1))
        elif kind == "ffn":
            f_wg = inp("ffn_w_gate", [2, D, DFF]); f_wu = inp("ffn_w_up", [2, D, DFF]); f_wd = inp("ffn_w_down", [2, DFF, D])
            phase(lambda C: ffn_phase(C, src, dst, nf[i], [(f_wg[j], f_wu[j], f_wd[j])], None,
                                      out_key="xo", rowmask=rowmask, after=None if last else gather(C)))
        elif kind == "moe":
            e_rt = inp("moe_router", [2, D, NE]); e_wg = inp("moe_w_gate", [2, NE, D, DFF])
            e_wu = inp("moe_w_up", [2, NE, D, DFF]); e_wd = inp("moe_w_down", [2, NE, DFF, D])
            phase(lambda C: ffn_phase(C, src, dst, nf[i],
                                      [(e_wg[j][e], e_wu[j][e], e_wd[j][e]) for e in range(NE)], e_rt[j],
                                      out_key="xo", rowmask=rowmask, after=None if last else gather(C)))
    nc._decl_inputs = set(decl)
    return nc


def _own_rows(r):
    return np.concatenate([np.arange((2 * j + r) * 128, (2 * j + r + 1) * 128) for j in range(NTO)])


def kernel(x, meta, norm_mixer, norm_ffn, dsa_w_in, dsa_q_norm, dsa_k_norm, dsa_w_out,
           mlstm_w_in, mlstm_b_i, mlstm_b_f, mlstm_out_norm, mlstm_w_out,
           ffn_w_gate, ffn_w_up, ffn_w_down, moe_router, moe_w_gate, moe_w_up, moe_w_down):
    global _PROG
    f = lambda a: np.ascontiguousarray(np.asarray(a), dtype=np.float32)
    x, meta = f(x), f(meta)
    B = x.shape[0]
    h = np.zeros((B, LP, D), np.float32)
    h[:, :NMETA] = meta[None]
    h[:, NMETA:LTOK] = x
    pos = np.arange(LP)
    c64, s64 = _rope_tabs(pos, 32)
    c128, s128 = _rope_tabs(pos, 64)
    s_ = np.arange(128)[:, None]
    t_ = np.arange(128)[None, :]
    shared = dict(norm_mixer=f(norm_mixer), norm_ffn=f(norm_ffn), dsa_w_in=f(dsa_w_in), dsa_q_norm=f(dsa_q_norm),
                  dsa_k_norm=f(dsa_k_norm), dsa_w_out=f(dsa_w_out), mlstm_w_in=f(mlstm_w_in),
                  mlstm_b_i=f(mlstm_b_i), mlstm_b_f=f(mlstm_b_f), mlstm_out_norm=f(mlstm_out_norm),
                  mlstm_w_out=f(mlstm_w_out), ffn_w_gate=f(ffn_w_gate), ffn_w_up=f(ffn_w_up),
                  ffn_w_down=f(ffn_w_down), moe_router=f(moe_router), moe_w_gate=f(moe_w_gate),
                  moe_w_up=f(moe_w_up), moe_w_down=f(moe_w_down), tk0=c64, tk1=s64, tk2=c128, tk3=s128)
    per_r = []
    for r in range(2):
        M0 = (s_ <= t_ + 128 * r).astype(np.float32)
        M1 = (128 + s_ <= t_ + 128 * r).astype(np.float32)
        pen = np.ascontiguousarray(np.where(np.concatenate([M0, M1], 0).T > 0, 0.0, -3e38).astype(np.float32))
        rows = _own_rows(r)
        rowmask = (rows[-128:] < LTOK).astype(np.float32)[:, None]
        per_r.append(dict(M0=M0, M1=M1, pen=pen, rowmask=np.ascontiguousarray(rowmask),
                          tq0=np.ascontiguousarray(c64[rows]), tq1=np.ascontiguousarray(s64[rows]),
                          tq2=np.ascontiguousarray(c128[rows]), tq3=np.ascontiguousarray(s128[rows]), rows=rows))
    maps = []
    for c in range(NCORES):
        b, r = c // 2, c % 2
        m = dict(shared)
        m.update({k: v for k, v in per_r[r].items() if k != "rows"})
        m["x0"] = np.ascontiguousarray(h[b][per_r[r]["rows"]])
        maps.append(m)
    if _PROG is None:
        _PROG = _build_fused(_PLAN)
    maps = [{k: v for k, v in m.items() if k in _PROG._decl_inputs} for m in maps]
    res = run_bass_kernel_spmd(_PROG, maps, core_ids=list(range(NCORES)))
    hn = np.empty_like(h)
    for c in range(NCORES):
        hn[c // 2][per_r[c % 2]["rows"]] = res.results[c]["out"]
    return np.ascontiguousarray(hn[:, NMETA:LTOK])
```
